# Optimizing a Trainium2 kernel written in Bass

```python
import math
import jax
import jax.numpy as jnp
from jax import lax
import numpy as np


D_MODEL = 2048
BATCH = 8
SEQ = 4096
DEPTH = 4

CHUNK = 64
N_MIXERS = 2
N_S5 = len(range(0, DEPTH, N_MIXERS))
N_HG = len(range(1, DEPTH, N_MIXERS))
N_ADA = 6
EPS = 1e-6
S5_GROUP = 16
S5_GROUPS = D_MODEL // S5_GROUP
S5_STATE = 64
S5_DT_MIN = 0.001
S5_DT_MAX = 0.1
S5_RE_MAX = -1e-4
HG_EXPAND = 128
HG_HEADS = D_MODEL // HG_EXPAND
HG_DK = HG_EXPAND
HG_FDIM = HG_HEADS * HG_DK
HG_DV = D_MODEL // HG_HEADS
HG_VDIM = HG_HEADS * HG_DV
N_EXPERTS = 32
TOP_K = 4
EXPERT_FF = D_MODEL // 4
SWIGLU_LIMIT = 7.0
SWIGLU_ALPHA = 1.702
MOE_BLOCK = 256

kernel_name = 'hybrid_s5_hgrn2_moe_adaln'


def rms_norm(x, g):
    xf = x.astype(jnp.float32)
    y = xf * lax.rsqrt(jnp.mean(xf * xf, axis=-1, keepdims=True) + EPS)
    return (y * g.astype(jnp.float32)).astype(x.dtype)


def s5_mixer(h, lam_re, lam_im, log_dt, b_re, b_im, c_re, c_im, d_skip, w_glu):
    bsz, seq, dm = h.shape
    nc = seq // CHUNK
    f32 = jnp.float32
    u = h.astype(f32)
    lam = lax.complex(jnp.minimum(lam_re.astype(f32), S5_RE_MAX), lam_im.astype(f32))
    dt = jnp.exp(log_dt.astype(f32))[:, None]
    lam_bar = jnp.exp(lam * dt)
    b_bar = ((lam_bar - 1.0) / lam)[:, :, None] * lax.complex(b_re.astype(f32), b_im.astype(f32))
    c_mat = lax.complex(c_re.astype(f32), c_im.astype(f32))
    steps = jnp.arange(1, CHUNK + 1, dtype=f32)[:, None, None]
    lam_pow = jnp.exp(lam[None] * (dt[None] * steps))
    uc = u.reshape(bsz, nc, CHUNK, S5_GROUPS, S5_GROUP).transpose(1, 0, 2, 3, 4)

    def combine(e1, e2):
        a1, s1 = e1
        a2, s2 = e2
        return a1 * a2, a2 * s1 + s2

    def step(state, u_t):
        bu = jnp.einsum('bcgh,gph->bcgp', u_t.astype(jnp.complex64), b_bar)
        a = jnp.broadcast_to(lam_bar, bu.shape)
        _, s_loc = lax.associative_scan(combine, (a, bu), axis=1)
        s_all = s_loc + lam_pow[None] * state[:, None]
        y = jnp.einsum('bcgp,ghp->bcgh', s_all, c_mat).real
        return s_all[:, -1], y

    s0 = jnp.zeros((bsz, S5_GROUPS, S5_STATE), jnp.complex64)
    _, y = lax.scan(step, s0, uc)
    y = y.transpose(1, 0, 2, 3, 4).reshape(bsz, seq, dm) + d_skip.astype(f32) * u
    z = jax.nn.gelu(y).astype(h.dtype)
    ga = z @ w_glu
    return ga[..., :dm] * jax.nn.sigmoid(ga[..., dm:])


def hgrn2_mixer(h, w_in, lb, g_norm, w_out):
    bsz, seq, dm = h.shape
    nc = seq // CHUNK
    f32 = jnp.float32
    proj = h @ w_in
    q = proj[..., :HG_FDIM]
    f_logit = proj[..., HG_FDIM:2 * HG_FDIM]
    v = proj[..., 2 * HG_FDIM:2 * HG_FDIM + HG_VDIM]
    gate = proj[..., 2 * HG_FDIM + HG_VDIM:]
    lb = lb.astype(f32)
    log_f = jnp.logaddexp(jnp.log(lb), jnp.log1p(-lb) + jax.nn.log_sigmoid(f_logit.astype(f32)))
    k = -jnp.expm1(log_f)

    def to_chunks(t, d):
        return t.astype(f32).reshape(bsz, nc, CHUNK, HG_HEADS, d).transpose(1, 0, 3, 2, 4)

    qc, kc, vc, gc = to_chunks(q, HG_DK), to_chunks(k, HG_DK), to_chunks(v, HG_DV), to_chunks(log_f, HG_DK)
    causal = jnp.tril(jnp.ones((CHUNK, CHUNK), dtype=bool))

    def step(state, inp):
        q_t, k_t, v_t, lf = inp
        b = jnp.cumsum(lf, axis=2)
        rel = jnp.where(causal[:, :, None], b[:, :, :, None, :] - b[:, :, None, :, :], -jnp.inf)
        scores = jnp.einsum('bhtd,bhsd,bhtsd->bhts', q_t, k_t, jnp.exp(rel))
        o = jnp.einsum('bhts,bhsv->bhtv', scores, v_t) + jnp.einsum('bhtd,bhdv->bhtv', q_t * jnp.exp(b), state)
        b_end = b[:, :, -1:, :]
        state = jnp.exp(b_end[:, :, 0, :, None]) * state + jnp.einsum('bhsd,bhsv->bhdv', k_t * jnp.exp(b_end - b), v_t)
        return state, o

    s0 = jnp.zeros((bsz, HG_HEADS, HG_DK, HG_DV), f32)
    _, o = lax.scan(step, s0, (qc, kc, vc, gc))
    o = o.transpose(1, 0, 3, 2, 4).reshape(bsz, seq, HG_HEADS, HG_DV)
    o = o * lax.rsqrt(jnp.mean(o * o, axis=-1, keepdims=True) + EPS) * g_norm.astype(f32)
    o = o * jax.nn.silu(gate.astype(f32).reshape(bsz, seq, HG_HEADS, HG_DV))
    return o.reshape(bsz, seq, dm).astype(h.dtype) @ w_out


def moe_ffn(h, w_r, b_r, w_gu, b_gu, w_dn, b_dn):
    bsz, seq, dm = h.shape
    n_tok = bsz * seq
    xt = h.reshape(n_tok, dm)
    logits = (xt @ w_r).astype(jnp.float32) + b_r.astype(jnp.float32)
    top_val, top_idx = lax.top_k(logits, TOP_K)
    gates = jax.nn.softmax(top_val, axis=-1)
    e_flat = top_idx.reshape(-1)
    tok_flat = jnp.repeat(jnp.arange(n_tok, dtype=jnp.int32), TOP_K)
    w_flat = gates.reshape(-1)
    onehot = jax.nn.one_hot(e_flat, N_EXPERTS, dtype=jnp.int32)
    counts = jnp.sum(onehot, axis=0)
    rank = jnp.take_along_axis(jnp.cumsum(onehot, axis=0), e_flat[:, None], axis=1)[:, 0] - 1
    padded = (counts + MOE_BLOCK - 1) // MOE_BLOCK * MOE_BLOCK
    pad_end = jnp.cumsum(padded)
    dest = (pad_end - padded)[e_flat] + rank
    n_blocks = -(-(n_tok * TOP_K) // MOE_BLOCK) + N_EXPERTS
    n_rows = n_blocks * MOE_BLOCK
    row_tok = jnp.zeros((n_rows,), jnp.int32).at[dest].set(tok_flat)
    row_w = jnp.zeros((n_rows,), jnp.float32).at[dest].set(w_flat)
    block_e = jnp.minimum(jnp.searchsorted(pad_end, jnp.arange(n_blocks, dtype=jnp.int32) * MOE_BLOCK, side='right'), N_EXPERTS - 1)

    def block_step(acc, blk):
        tok, wt, e = blk
        gu = xt[tok] @ w_gu[e] + b_gu[e]
        gt = jnp.minimum(gu[:, :EXPERT_FF], SWIGLU_LIMIT)
        up = jnp.clip(gu[:, EXPERT_FF:], -SWIGLU_LIMIT, SWIGLU_LIMIT)
        act = (up + 1.0) * gt * jax.nn.sigmoid(SWIGLU_ALPHA * gt)
        y = (act @ w_dn[e] + b_dn[e]).astype(jnp.float32) * wt[:, None]
        return acc.at[tok].add(y), None

    acc, _ = lax.scan(block_step, jnp.zeros((n_tok, dm), jnp.float32),
                      (row_tok.reshape(n_blocks, MOE_BLOCK), row_w.reshape(n_blocks, MOE_BLOCK), block_e))
    return acc.reshape(bsz, seq, dm).astype(h.dtype)


def setup_inputs(seed: int = 0) -> dict:
    key = jax.random.key(seed)
    ks = jax.random.split(key, 32)
    f32 = jnp.float32
    D = D_MODEL
    G, P, H = S5_GROUPS, S5_STATE, S5_GROUP
    E, F = N_EXPERTS, EXPERT_FF

    def nrm(k, shape, scale):
        return jax.random.normal(k, shape, f32) * scale

    x = nrm(ks[0], (BATCH, SEQ, D), 1.0)
    c = nrm(ks[1], (BATCH, D), 1.0)
    ada_w = nrm(ks[2], (DEPTH, D, N_ADA * D), 0.5 * D ** -0.5)
    ada_b = nrm(ks[3], (DEPTH, N_ADA * D), 0.02)
    norm_mix = 1.0 + nrm(ks[4], (DEPTH, D), 0.02)
    norm_ffn = 1.0 + nrm(ks[5], (DEPTH, D), 0.02)
    norm_final = 1.0 + nrm(ks[6], (D,), 0.02)
    s5_lambda_re = -0.5 + nrm(ks[7], (N_S5, G, P), 0.01)
    s5_lambda_im = math.pi * jnp.arange(P, dtype=f32) + nrm(ks[8], (N_S5, G, P), 0.01)
    s5_log_dt = jax.random.uniform(ks[9], (N_S5, G), f32, math.log(S5_DT_MIN), math.log(S5_DT_MAX))
    s5_b_re = nrm(ks[10], (N_S5, G, P, H), (2 * H) ** -0.5)
    s5_b_im = nrm(ks[11], (N_S5, G, P, H), (2 * H) ** -0.5)
    s5_c_re = nrm(ks[12], (N_S5, G, H, P), P ** -0.5)
    s5_c_im = nrm(ks[13], (N_S5, G, H, P), P ** -0.5)
    s5_d = nrm(ks[14], (N_S5, D), 1.0)
    s5_w_glu = nrm(ks[15], (N_S5, D, 2 * D), D ** -0.5)
    hg_w_in = nrm(ks[16], (N_HG, D, 2 * HG_FDIM + 2 * HG_VDIM), D ** -0.5)
    hg_lb_raw = nrm(ks[17], (DEPTH, HG_FDIM), 0.1)
    hg_norm = 1.0 + nrm(ks[18], (N_HG, HG_DV), 0.02)
    hg_w_out = nrm(ks[19], (N_HG, HG_VDIM, D), HG_VDIM ** -0.5)
    router_w = nrm(ks[20], (DEPTH, D, E), D ** -0.5)
    router_b = nrm(ks[21], (DEPTH, E), 0.01)
    moe_w_gate_up = nrm(ks[22], (DEPTH, E, D, 2 * F), D ** -0.5)
    moe_b_gate_up = nrm(ks[23], (DEPTH, E, 2 * F), 0.01)
    moe_w_down = nrm(ks[24], (DEPTH, E, F, D), F ** -0.5)
    moe_b_down = nrm(ks[25], (DEPTH, E, D), 0.01)
    return {'x': x, 'c': c, 'ada_w': ada_w, 'ada_b': ada_b,
            'norm_mix': norm_mix, 'norm_ffn': norm_ffn, 'norm_final': norm_final,
            's5_lambda_re': s5_lambda_re, 's5_lambda_im': s5_lambda_im, 's5_log_dt': s5_log_dt,
            's5_b_re': s5_b_re, 's5_b_im': s5_b_im, 's5_c_re': s5_c_re, 's5_c_im': s5_c_im,
            's5_d': s5_d, 's5_w_glu': s5_w_glu,
            'hg_w_in': hg_w_in, 'hg_lb_raw': hg_lb_raw, 'hg_norm': hg_norm, 'hg_w_out': hg_w_out,
            'router_w': router_w, 'router_b': router_b,
            'moe_w_gate_up': moe_w_gate_up, 'moe_b_gate_up': moe_b_gate_up,
            'moe_w_down': moe_w_down, 'moe_b_down': moe_b_down}


def reference(x, c, ada_w, ada_b, norm_mix, norm_ffn, norm_final,
              s5_lambda_re, s5_lambda_im, s5_log_dt, s5_b_re, s5_b_im, s5_c_re, s5_c_im,
              s5_d, s5_w_glu, hg_w_in, hg_lb_raw, hg_norm, hg_w_out,
              router_w, router_b, moe_w_gate_up, moe_b_gate_up, moe_w_down, moe_b_down):
    lb_p = jax.nn.softmax(hg_lb_raw.astype(jnp.float32), axis=0)
    lb_all = jnp.cumsum(lb_p, axis=0) - lb_p[0]
    c_act = jax.nn.silu(c)
    for i in range(DEPTH):
        mod = (c_act @ ada_w[i] + ada_b[i])[:, None, :]
        sh1, sc1, g1, sh2, sc2, g2 = jnp.split(mod, N_ADA, axis=-1)
        h = rms_norm(x, norm_mix[i]) * (1.0 + sc1) + sh1
        j = i // N_MIXERS
        if i % N_MIXERS == 0:
            y = s5_mixer(h, s5_lambda_re[j], s5_lambda_im[j], s5_log_dt[j], s5_b_re[j], s5_b_im[j],
                         s5_c_re[j], s5_c_im[j], s5_d[j], s5_w_glu[j])
        else:
            y = hgrn2_mixer(h, hg_w_in[j], lb_all[i], hg_norm[j], hg_w_out[j])
        x = x + g1 * y
        h = rms_norm(x, norm_ffn[i]) * (1.0 + sc2) + sh2
        x = x + g2 * moe_ffn(h, router_w[i], router_b[i], moe_w_gate_up[i], moe_b_gate_up[i],
                             moe_w_down[i], moe_b_down[i])
    return rms_norm(x, norm_final)
```

```python
import contextlib
import math
import numpy as np
import concourse.bass as bass
import concourse.mybir as mybir
from concourse.bass_utils import run_bass_kernel_spmd

F32 = mybir.dt.float32
BF16 = mybir.dt.bfloat16
AF = mybir.ActivationFunctionType
ALU = mybir.AluOpType
AX = mybir.AxisListType

D = 2048
NCH = 16
NE = 32
FF = 512
DEPTH = 4
EPS = 1e-6
TB = 512
MAGIC = 12582912.0
TWO_PI = 2.0 * math.pi
SEM_LIMIT = 30000


ALL_BUFS = []


class Buf:
    __slots__ = ("w", "r", "frozen")

    def __init__(self):
        self.w = None
        self.r = []
        self.frozen = False
        ALL_BUFS.append(self)


class T:
    def __init__(self, t):
        self.t = t
        self.b = Buf()

    def __getitem__(self, k):
        return self.t[k]


class Sched:
    def __init__(self, nc, es):
        self.nc = nc
        self.es = es
        self.ops = []
        self.last_op = {}
        self.engs = {"pe": nc.tensor, "act": nc.scalar, "dve": nc.vector, "pool": nc.gpsimd, "sp": nc.sync}

    def op(self, eng, fn, reads=(), writes=(), dma=False):
        idx = len(self.ops)
        deps = set()
        for b in reads:
            b = b.b if isinstance(b, T) else b
            if b.w is not None:
                deps.add(b.w)
        for b in writes:
            b = b.b if isinstance(b, T) else b
            if b.w is not None:
                deps.add(b.w)
            deps.update(b.r)
        for b in reads:
            b = b.b if isinstance(b, T) else b
            if not b.frozen:
                b.r.append(idx)
        for b in writes:
            b = b.b if isinstance(b, T) else b
            b.w = idx
            b.r = []
        self.ops.append((eng, fn, deps, dma))
        self.last_op[eng] = idx
        return idx

    def _init_emit(self):
        nc, es = self.nc, self.es
        self.done = 0
        self.sem_of = {}
        self.cur = {}
        self.dma_pool = [es.enter_context(nc.semaphore(f"dq{i}")) for i in range(32)]
        self.dma_cnt = [0] * len(self.dma_pool)
        self.dma_rr = 0
        self.retired = []
        self.waited = {e: {} for e in self.engs}
        self.nsem = 0

    def flush(self):
        nc, es = self.nc, self.es
        if not hasattr(self, "done"):
            self._init_emit()
        ops = self.ops
        n = len(ops)
        lo = self.done
        needs = set()
        for i in range(lo, n):
            eng, fn, deps, dma = ops[i]
            for d in deps:
                deng, _, _, ddma = ops[d]
                if ddma or deng != eng or eng != "pe":
                    needs.add(d)
        for eng_, li_ in self.last_op.items():
            if li_ >= lo and not ops[li_][3]:
                needs.add(li_)
        for b in ALL_BUFS:
            if b.w is not None:
                needs.add(b.w)
            needs.update(b.r)
        sem_of = self.sem_of
        for i in range(lo, n):
            eng, fn, deps, dma = ops[i]
            h = self.engs[eng]
            w = self.waited[eng]
            pend = {}
            for d in deps:
                deng, _, _, ddma = ops[d]
                if (not ddma) and deng == eng and eng == "pe":
                    continue
                s, v = sem_of[d]
                if w.get(s, 0) < v and pend.get(s, 0) < v:
                    pend[s] = v
            if dma:
                k = self.dma_rr
                self.dma_rr = (self.dma_rr + 1) % len(self.dma_pool)
                if self.dma_cnt[k] + 16 > SEM_LIMIT:
                    self.retired.append((self.dma_pool[k], self.dma_cnt[k]))
                    self.dma_pool[k] = es.enter_context(nc.semaphore(f"dq{k}_{i}"))
                    self.dma_cnt[k] = 0
                dsem = self.dma_pool[k]
                if self.dma_cnt[k] > 0 and w.get(dsem, 0) < self.dma_cnt[k]:
                    pend[dsem] = max(pend.get(dsem, 0), self.dma_cnt[k])
            for ws, wv in pend.items():
                h.wait_ge(ws, wv)
                w[ws] = wv
            ins = fn()
            if dma:
                self.dma_cnt[k] += 16
                ins.then_inc(dsem, 16)
                sem_of[i] = (dsem, self.dma_cnt[k])
            elif i in needs:
                c = self.cur.get(eng)
                if c is None or c[1] + 1 > SEM_LIMIT:
                    c = [es.enter_context(nc.semaphore(f"e{eng}{self.nsem}")), 0]
                    self.nsem += 1
                    self.cur[eng] = c
                c[1] += 1
                ins.then_inc(c[0], 1)
                sem_of[i] = (c[0], c[1])
            ops[i] = (eng, None, deps, dma)
        self.done = n
        evs = []
        for eng_, li_ in self.last_op.items():
            if li_ in sem_of:
                evs.append(sem_of[li_])
        for k, s_ in enumerate(self.dma_pool):
            if self.dma_cnt[k] > 0:
                evs.append((s_, self.dma_cnt[k]))
        evs.extend(self.retired)
        for eng_, h in self.engs.items():
            w = self.waited[eng_]
            for s_, v_ in evs:
                if w.get(s_, 0) < v_:
                    h.wait_ge(s_, v_)
                    w[s_] = v_
        return n

    def finish(self):
        nc = self.nc
        self.flush()
        for k, s in enumerate(self.dma_pool):
            if self.dma_cnt[k] > 0:
                nc.sync.wait_ge(s, self.dma_cnt[k])
        for s, c in self.retired:
            nc.sync.wait_ge(s, c)
        for eng, c in self.cur.items():
            nc.sync.wait_ge(c[0], c[1])


def _layout():
    off = {}
    n = 0

    def add(name, w):
        nonlocal n
        off[name] = n
        n += w

    add("cT", NCH)
    add("ada_b", DEPTH * 96)
    add("norm_mix", DEPTH * NCH)
    add("norm_ffn", DEPTH * NCH)
    add("norm_final", NCH)
    add("s5_d", 2 * NCH)
    add("lb_raw", DEPTH * NCH)
    add("hg_norm", 2)
    add("b_gu", DEPTH * NE * 8)
    add("s5_lre", 2 * 128)
    add("s5_lim", 2 * 128)
    add("s5_ldt", 2 * 128)
    off["_n"] = n
    return off


VOFF = _layout()


def _consts():
    c = {}
    c["ident"] = np.eye(128, dtype=np.float32)
    c["ones"] = np.ones((128, 128), np.float32)
    J = np.zeros((128, 128), np.float32)
    for p in range(64):
        J[64 + p, p] = -1.0
        J[p, 64 + p] = 1.0
    c["J"] = J
    gm = np.zeros((128, 8), np.float32)
    for p in range(128):
        gm[p, p // 16] = 1.0
    c["gmask"] = gm
    sg = np.ones((128, 1), np.float32)
    sg[64:] = -1.0
    c["sgn"] = sg
    c["iota"] = np.tile(np.arange(TB + 1, dtype=np.float32)[None, :], (128, 1))
    c["cmask"] = np.tile((np.arange(TB) % 64 != 0).astype(np.float32)[None, :], (128, 1))
    s = np.arange(128)[:, None]
    t = np.arange(128)[None, :]
    c["bcmask"] = ((s // 64 == t // 64) & (s <= t)).astype(np.float32)
    names = ["ident", "ones", "J", "gmask", "sgn", "iota", "cmask", "bcmask"]
    offs = {}
    n = 0
    for k in names:
        offs[k] = n
        n += c[k].shape[1]
    return np.concatenate([c[k] for k in names], axis=1), offs


CONST_NP, COFF = _consts()


def build(L, layers=tuple(range(DEPTH)), dbg=()):
    NB = L // TB
    del ALL_BUFS[:]
    nc = bass.Bass("TRN2", target_bir_lowering=False)
    dram = {}
    dbufs = {}

    def din(name, shape, dt=F32):
        dram[name] = nc.dram_tensor(name, list(shape), dt, kind="ExternalInput").ap()
        return dram[name]

    def dscr(name, shape, dt=F32):
        kind = "ExternalOutput" if name in dbg else "Internal"
        dram[name] = nc.dram_tensor(name, list(shape), dt, kind=kind).ap()
        return dram[name]

    def DB(*key):
        if key not in dbufs:
            dbufs[key] = Buf()
        return dbufs[key]

    xin = din("xT", [NCH, 128, L])
    vec = din("vec", [128, VOFF["_n"]])
    cst = din("cst", [128, CONST_NP.shape[1]])
    NL = len(layers)
    LI = {l: n_ for n_, l in enumerate(layers)}
    nomoe = "nomoe" in dbg
    ada_w = din("ada_w", [NL, D, 6 * D])
    s5B = din("s5B", [2, 2, 128, 128 * 16])
    s5C = din("s5C", [2, 2, 128, 128 * 16])
    w_glu = din("s5_w_glu", [2, D, 2 * D])
    w_in = din("hg_w_in", [2, D, 4 * D])
    w_out = din("hg_w_out", [2, D, D])
    router_w = din("router_w", [DEPTH, D, NE])
    router_b = din("router_b", [1, DEPTH * NE])
    w_gu = din("moe_w_gate_up", [1, 1, 1, 1] if nomoe else [NL, NE, D, 2 * FF])
    w_dn = din("moe_w_down", [1, 1, 1, 1] if nomoe else [NL, NE, FF, D])
    b_dn = din("moe_b_down", [1, 1, 1] if nomoe else [NL, NE, D])
    out = nc.dram_tensor("outT", [NCH, 128, L], F32, kind="ExternalOutput").ap()

    xT = dscr("xTs", [NCH, 128, L])
    actT = dscr("actT", [NCH, 128, L], BF16)
    hT = dscr("hT", [NCH, 128, L], BF16)
    ymoe = dscr("ymoe", [NCH, 128, L])

    with contextlib.ExitStack() as es:
        S = Sched(nc, es)

        @contextlib.contextmanager
        def scope():
            with contextlib.ExitStack() as e:
                yield e
                S.flush()

        uid = [0]

        def sb(es_, name, shape, dt=F32):
            uid[0] += 1
            return T(es_.enter_context(nc.sbuf_tensor(f"{name}_{uid[0]}", list(shape), dt)))

        def ps(es_, name, shape=(128, 512), dt=F32):
            uid[0] += 1
            return T(es_.enter_context(nc.psum_tensor(f"{name}_{uid[0]}", [128, 512 if dt == F32 else 1024], dt)))

        def dma(q, out_ap, in_ap, reads, writes, **kw):
            h = {"sp": nc.sync, "pool": nc.gpsimd}[q]
            S.op(q, lambda: h.dma_start(out=out_ap, in_=in_ap, **kw), reads=reads, writes=writes, dma=True)

        def dump(name, t, shape, ap=None):
            if ("dump" not in dbg) or name in dram:
                return
            d = nc.dram_tensor("dbg_" + name, list(shape), F32, kind="ExternalOutput").ap()
            dram[name] = d
            dma("sp", d[:, :], t[:] if ap is None else ap, [t], [Buf()])

        V = sb(es, "V", [128, VOFF["_n"]])
        C = sb(es, "C", [128, CONST_NP.shape[1]])
        dma("sp", V[:], vec[:, :], [], [V])
        dma("sp", C[:], cst[:, :], [], [C])

        def vcol(name, i=0, w=1):
            o = VOFF[name] + i
            return V[:, o:o + w]

        def ccol(name, i=0, w=None):
            o = COFF[name] + i
            if w is None:
                w = {"ident": 128, "ones": 128, "J": 128, "gmask": 8, "sgn": 1, "iota": TB + 1, "cmask": TB,
                     "bcmask": 128}[name] - i
            return C[:, o:o + w]

        ident = ccol("ident")
        ones = ccol("ones")
        ZB = sb(es, "ZB", [128, 512], BF16)
        S.op("dve", lambda: nc.vector.memset(ZB[:], 0.0), writes=[ZB])

        def fence(out_ap, M, N, reads, writes):
            S.op("pe", lambda: nc.tensor.matmul(out_ap, lhsT=ZB[:, 0:M], rhs=ZB[:, 0:N], start=False, stop=True),
                 reads=list(reads) + [ZB], writes=writes)

        MOD = sb(es, "MOD", [128, DEPTH * 96])
        AB = sb(es, "AB", [128, DEPTH * 64])
        LB = sb(es, "LB", [128, DEPTH * NCH * 2])
        ones_bf = sb(es, "ones_bf", [128, 128], BF16)
        onesD = sb(es, "onesD", [128, 128])
        S.op("dve", lambda: nc.vector.memset(onesD[:], 1.0 / 128.0), writes=[onesD])

        with scope() as e1:
            xb = [sb(e1, f"xcp{i}", [128, L]) for i in range(2)]
            for c in range(NCH):
                t = xb[c % 2]
                dma("sp", t[:], xin[c, :, :], [], [t])
                dma("sp", xT[c, :, :], t[:], [t], [DB("x", c)])

        with scope() as e1:
            cact = sb(e1, "cact", [128, NCH])
            S.op("act", lambda: nc.scalar.activation(out=cact[:], in_=vcol("cT", 0, NCH), func=AF.Silu),
                 reads=[V], writes=[cact])
            wt = [sb(e1, f"adaw{i}", [128, NCH, 512]) for i in range(2)]
            mps = ps(e1, "modps", [128, 512])
            nld = 0
            for i in layers:
                for cb in range(24):
                    w = wt[nld % 2]
                    nld += 1
                    src = ada_w[LI[i], :, cb * 512:(cb + 1) * 512].rearrange("(k p) n -> p k n", p=128)
                    dma("sp", w[:], src, [], [w])
                    for q in range(4):
                        col = cb * 4 + q
                        for k in range(NCH):
                            S.op("pe", (lambda w=w, q=q, k=k, col=col: nc.tensor.matmul(
                                mps[:, col:col + 1], lhsT=w[:, k, q * 128:(q + 1) * 128], rhs=cact[:, k:k + 1],
                                start=(k == 0), stop=(k == NCH - 1))), reads=[w, cact], writes=[mps])
                fence(mps[:, 95:96], 128, 1, [], [mps])
                S.op("dve", (lambda i=i: nc.vector.tensor_tensor(
                    out=MOD[:, i * 96:(i + 1) * 96], in0=mps[:, 0:96], in1=vcol("ada_b", i * 96, 96), op=ALU.add)),
                     reads=[mps, V], writes=[MOD])
                dump("cact", cact, [128, NCH]); dump("wlast", w, [128, NCH * 512], w[:].rearrange("p k n -> p (k n)"))
                dump("MOD0", MOD, [128, DEPTH * 96])
                for j, nm in ((0, "norm_mix"), (1, "norm_ffn")):
                    S.op("dve", (lambda i=i, j=j, nm=nm: nc.vector.scalar_tensor_tensor(
                        out=AB[:, i * 64 + j * 16: i * 64 + j * 16 + 16],
                        in0=MOD[:, i * 96 + (1 + 3 * j) * 16: i * 96 + (1 + 3 * j) * 16 + 16], scalar=1.0,
                        in1=vcol(nm, i * NCH, NCH), op0=ALU.add, op1=ALU.mult)), reads=[MOD, V], writes=[AB])

        def modc(i, j, c):
            o = i * 96 + j * 16 + c
            return MOD[:, o:o + 1]

        def acol(i, j, c):
            o = i * 64 + j * 16 + c
            return AB[:, o:o + 1]

        with scope() as e1:
            ex = sb(e1, "lbex", [128, DEPTH * NCH])
            mx = sb(e1, "lbmx", [128, NCH])
            sm = sb(e1, "lbsm", [128, NCH])
            raw = lambda l: vcol("lb_raw", l * NCH, NCH)
            S.op("dve", lambda: nc.vector.tensor_max(out=mx[:], in0=raw(0), in1=raw(1)), reads=[V], writes=[mx])
            S.op("dve", lambda: nc.vector.tensor_max(out=mx[:], in0=mx[:], in1=raw(2)), reads=[V, mx], writes=[mx])
            S.op("dve", lambda: nc.vector.tensor_max(out=mx[:], in0=mx[:], in1=raw(3)), reads=[V, mx], writes=[mx])
            for l in range(DEPTH):
                S.op("dve", (lambda l=l: nc.vector.tensor_tensor(out=ex[:, l * NCH:(l + 1) * NCH], in0=raw(l), in1=mx[:],
                                                                 op=ALU.subtract)), reads=[V, mx], writes=[ex])
            S.op("act", lambda: nc.scalar.activation(out=ex[:], in_=ex[:], func=AF.Exp), reads=[ex], writes=[ex])
            S.op("dve", lambda: nc.vector.tensor_tensor(out=sm[:], in0=ex[:, 0:NCH], in1=ex[:, NCH:2 * NCH], op=ALU.add),
                 reads=[ex], writes=[sm])
            S.op("dve", lambda: nc.vector.tensor_tensor(out=sm[:], in0=sm[:], in1=ex[:, 2 * NCH:3 * NCH], op=ALU.add),
                 reads=[ex, sm], writes=[sm])
            S.op("dve", lambda: nc.vector.tensor_tensor(out=sm[:], in0=sm[:], in1=ex[:, 3 * NCH:4 * NCH], op=ALU.add),
                 reads=[ex, sm], writes=[sm])
            S.op("dve", lambda: nc.vector.reciprocal(out=sm[:], in_=sm[:]), reads=[sm], writes=[sm])
            for l in range(DEPTH):
                S.op("dve", (lambda l=l: nc.vector.tensor_tensor(out=ex[:, l * NCH:(l + 1) * NCH],
                                                                 in0=ex[:, l * NCH:(l + 1) * NCH], in1=sm[:], op=ALU.mult)),
                     reads=[ex, sm], writes=[ex])
            S.op("dve", lambda: nc.vector.memset(LB[:, 0:NCH], 0.0), writes=[LB])
            for l in range(1, DEPTH):
                S.op("dve", (lambda l=l: nc.vector.tensor_tensor(out=LB[:, l * NCH:(l + 1) * NCH],
                                                                 in0=LB[:, (l - 1) * NCH:l * NCH],
                                                                 in1=ex[:, l * NCH:(l + 1) * NCH], op=ALU.add)),
                     reads=[LB, ex], writes=[LB])
            S.op("dve", lambda: nc.vector.tensor_scalar(LB[:, DEPTH * NCH:2 * DEPTH * NCH], LB[:, 0:DEPTH * NCH], -1.0, 1.0,
                                                        op0=ALU.mult, op1=ALU.add), reads=[LB], writes=[LB])

        FREEZE = [V, C, MOD, AB, LB, onesD]
        def rstd_phase(e1, src):
            rstd = sb(e1, "rstd", [128, L])
            with scope() as e2:
                xt = [sb(e2, f"rs_x{i}", [128, TB]) for i in range(3)]
                sq = [sb(e2, f"rs_sq{i}", [128, TB]) for i in range(2)]
                pss = [ps(e2, f"rs_ps{i}") for i in range(2)]
                tmp = sb(e2, "rs_tmp", [128, TB])
                n = 0
                for blk in range(NB):
                    p = pss[blk % 2]
                    for c in range(NCH):
                        x = xt[n % 3]
                        q = sq[n % 2]
                        n += 1
                        dma("sp", x[:], src[c, :, blk * TB:(blk + 1) * TB], [DB("x", c)], [x])
                        S.op("act", (lambda x=x, q=q: nc.scalar.activation(out=q[:], in_=x[:], func=AF.Square)),
                             reads=[x], writes=[q])
                        S.op("pe", (lambda p=p, q=q, c=c: nc.tensor.matmul(p[:], lhsT=ones, rhs=q[:], start=(c == 0),
                                                                          stop=(c == NCH - 1))), reads=[q, C], writes=[p])
                    fence(p[:], 128, TB, [], [p])
                    S.op("act", (lambda p=p: nc.scalar.activation(out=tmp[:], in_=p[:], func=AF.Sqrt, scale=1.0 / D,
                                                                   bias=epsc[:, 0:1])), reads=[p, epsc], writes=[tmp])
                    S.op("dve", (lambda blk=blk: nc.vector.reciprocal(out=rstd[:, blk * TB:(blk + 1) * TB], in_=tmp[:])),
                         reads=[tmp], writes=[rstd])
            return rstd

        epsc = sb(es, "epsc", [128, 1])
        S.op("dve", lambda: nc.vector.memset(epsc[:], EPS), writes=[epsc])

        def s5_layer(i):
            j = i // 2
            with scope() as e1:
                rstd = rstd_phase(e1, xT)
                nm = ["lr", "li", "dt", "wr", "wi", "t0", "t1", "t2", "sn", "cs", "mg", "lbr", "lbi", "den", "cr", "ci",
                      "ncis", "ncrs", "nci"]
                cf = {k: sb(e1, "s5c_" + k, [128, 128]) for k in nm}
                lre = vcol("s5_lre", j * 128, 128)
                lim = vcol("s5_lim", j * 128, 128)
                ldt = vcol("s5_ldt", j * 128, 128)
                sgn = ccol("sgn")
                vop = lambda fn, r, w: S.op("dve", fn, reads=r, writes=w)
                vop(lambda: nc.vector.tensor_scalar(cf["lr"][:], lre, -1e-4, None, op0=ALU.min), [V], [cf["lr"]])
                S.op("act", lambda: nc.scalar.activation(out=cf["dt"][:], in_=ldt, func=AF.Exp), reads=[V], writes=[cf["dt"]])
                vop(lambda: nc.vector.tensor_tensor(out=cf["wr"][:], in0=cf["lr"][:], in1=cf["dt"][:], op=ALU.mult),
                    [cf["lr"], cf["dt"]], [cf["wr"]])
                vop(lambda: nc.vector.tensor_tensor(out=cf["wi"][:], in0=lim, in1=cf["dt"][:], op=ALU.mult),
                    [V, cf["dt"]], [cf["wi"]])

                def sincos(theta, sn_out, cs_out, shape_t, rd, tmp0, tmp1):
                    for shift, o in ((0.0, sn_out), (0.5 * math.pi, cs_out)):
                        vop(lambda shift=shift: nc.vector.tensor_scalar(tmp0, theta, shift, 1.0 / TWO_PI, op0=ALU.add,
                                                                         op1=ALU.mult), rd, shape_t)
                        vop(lambda: nc.vector.tensor_scalar(tmp1, tmp0, MAGIC, MAGIC, op0=ALU.add, op1=ALU.subtract),
                            shape_t, shape_t)
                        vop(lambda: nc.vector.tensor_tensor(out=tmp0, in0=tmp0, in1=tmp1, op=ALU.subtract), shape_t, shape_t)
                        S.op("act", (lambda o=o: nc.scalar.activation(out=o, in_=tmp0, func=AF.Sin, scale=TWO_PI * 0.999999)),
                             reads=shape_t, writes=shape_t)

                tl = [cf["t0"], cf["t1"], cf["sn"], cf["cs"]]
                sincos(cf["wi"][:], cf["sn"][:], cf["cs"][:], tl, [cf["wi"]] + tl, cf["t0"][:], cf["t1"][:])
                S.op("act", lambda: nc.scalar.activation(out=cf["mg"][:], in_=cf["wr"][:], func=AF.Exp),
                     reads=[cf["wr"]], writes=[cf["mg"]])
                vop(lambda: nc.vector.tensor_tensor(out=cf["lbr"][:], in0=cf["cs"][:], in1=cf["mg"][:], op=ALU.mult),
                    [cf["cs"], cf["mg"]], [cf["lbr"]])
                vop(lambda: nc.vector.tensor_tensor(out=cf["lbi"][:], in0=cf["sn"][:], in1=cf["mg"][:], op=ALU.mult),
                    [cf["sn"], cf["mg"]], [cf["lbi"]])
                vop(lambda: nc.vector.tensor_tensor(out=cf["den"][:], in0=cf["lr"][:], in1=cf["lr"][:], op=ALU.mult),
                    [cf["lr"]], [cf["den"]])
                vop(lambda: nc.vector.tensor_tensor(out=cf["t0"][:], in0=lim, in1=lim, op=ALU.mult), [V], [cf["t0"]])
                vop(lambda: nc.vector.tensor_tensor(out=cf["den"][:], in0=cf["den"][:], in1=cf["t0"][:], op=ALU.add),
                    [cf["den"], cf["t0"]], [cf["den"]])
                vop(lambda: nc.vector.reciprocal(out=cf["den"][:], in_=cf["den"][:]), [cf["den"]], [cf["den"]])
                vop(lambda: nc.vector.tensor_scalar(cf["t2"][:], cf["lbr"][:], -1.0, None, op0=ALU.add), [cf["lbr"]], [cf["t2"]])
                vop(lambda: nc.vector.tensor_tensor(out=cf["t0"][:], in0=cf["t2"][:], in1=cf["lr"][:], op=ALU.mult),
                    [cf["t2"], cf["lr"]], [cf["t0"]])
                vop(lambda: nc.vector.tensor_tensor(out=cf["t1"][:], in0=cf["lbi"][:], in1=lim, op=ALU.mult),
                    [cf["lbi"], V], [cf["t1"]])
                vop(lambda: nc.vector.tensor_tensor(out=cf["t0"][:], in0=cf["t0"][:], in1=cf["t1"][:], op=ALU.add),
                    [cf["t0"], cf["t1"]], [cf["t0"]])
                vop(lambda: nc.vector.tensor_tensor(out=cf["cr"][:], in0=cf["t0"][:], in1=cf["den"][:], op=ALU.mult),
                    [cf["t0"], cf["den"]], [cf["cr"]])
                vop(lambda: nc.vector.tensor_tensor(out=cf["t0"][:], in0=cf["lbi"][:], in1=cf["lr"][:], op=ALU.mult),
                    [cf["lbi"], cf["lr"]], [cf["t0"]])
                vop(lambda: nc.vector.tensor_tensor(out=cf["t1"][:], in0=cf["t2"][:], in1=lim, op=ALU.mult),
                    [cf["t2"], V], [cf["t1"]])
                vop(lambda: nc.vector.tensor_tensor(out=cf["t0"][:], in0=cf["t0"][:], in1=cf["t1"][:], op=ALU.subtract),
                    [cf["t0"], cf["t1"]], [cf["t0"]])
                vop(lambda: nc.vector.tensor_tensor(out=cf["ci"][:], in0=cf["t0"][:], in1=cf["den"][:], op=ALU.mult),
                    [cf["t0"], cf["den"]], [cf["ci"]])
                vop(lambda: nc.vector.tensor_scalar(cf["ncis"][:], cf["ci"][:], sgn, -1.0, op0=ALU.mult, op1=ALU.mult),
                    [cf["ci"], C], [cf["ncis"]])
                vop(lambda: nc.vector.tensor_scalar(cf["ncrs"][:], cf["cr"][:], sgn, None, op0=ALU.mult),
                    [cf["cr"], C], [cf["ncrs"]])
                vop(lambda: nc.vector.tensor_copy(out=cf["nci"][:], in_=cf["ci"][:]), [cf["ci"]], [cf["nci"]])
                dump("MOD", MOD, [128, DEPTH * 96]); dump("rstd", rstd, [128, L])
                for k_ in ("wr", "wi", "sn", "cs", "lbr", "lbi", "cr", "ci"):
                    dump("cf_" + k_, cf[k_], [128, 128])
                nwr = cf["den"]
                vop(lambda: nc.vector.tensor_scalar(nwr[:], cf["wr"][:], -1.0, None, op0=ALU.mult), [cf["wr"]], [nwr])

                with scope() as e2:
                    xc = [sb(e2, f"s5x{k}", [128, L]) for k in range(1)]
                    u32 = sb(e2, "s5u32", [128, L])
                    ub = sb(e2, "s5ub", [128, L], BF16)
                    yacc = sb(e2, "s5yacc", [128, L])
                    zb = [sb(e2, f"s5zb{k}", [128, L], BF16) for k in range(1)]
                    Bri = sb(e2, "s5Bri", [128, 128])
                    Bir = sb(e2, "s5Bir", [128, 128])
                    Cst = sb(e2, "s5Cst", [128, 128])
                    Csw = sb(e2, "s5Csw", [128, 128])
                    B1s = sb(e2, "s5B1s", [128, 128])
                    B2s = sb(e2, "s5B2s", [128, 128])
                    bt = sb(e2, "s5bt", [128, 16])
                    B1g = sb(e2, "s5B1g", [128, 8, 128], BF16)
                    B2g = sb(e2, "s5B2g", [128, 8, 128], BF16)
                    CaZ = sb(e2, "s5CaZ", [128, 8, 128], BF16)
                    CbZ = sb(e2, "s5CbZ", [128, 8, 128], BF16)
                    S.op("pool", lambda: nc.gpsimd.memset(CaZ[:], 0.0), writes=[CaZ])
                    S.op("pool", lambda: nc.gpsimd.memset(CbZ[:], 0.0), writes=[CbZ])
                    tabs = [{k: sb(e2, f"s5t{q}_{k}", [128, TB + 1]) for k in ("Ere", "Eim", "Fre", "Fim")} for q in range(2)]
                    th = sb(e2, "s5th", [128, TB + 1])
                    tq0 = sb(e2, "s5tq0", [128, TB + 1])
                    tq1 = sb(e2, "s5tq1", [128, TB + 1])
                    snt = sb(e2, "s5snt", [128, TB + 1])
                    cst_ = sb(e2, "s5cst", [128, TB + 1])
                    mgE = sb(e2, "s5mgE", [128, TB + 1])
                    mgF = sb(e2, "s5mgF", [128, TB + 1])
                    W1 = [sb(e2, f"s5W1{k}", [128, TB]) for k in range(2)]
                    W2 = [sb(e2, f"s5W2{k}", [128, TB]) for k in range(2)]
                    Pp = [sb(e2, f"s5P{k}", [128, TB]) for k in range(2)]
                    Q1 = [sb(e2, f"s5Q1{k}", [128, TB], BF16) for k in range(2)]
                    Q2 = [sb(e2, f"s5Q2{k}", [128, TB], BF16) for k in range(2)]
                    onesT = sb(e2, "s5ones", [128, TB])
                    S.op("pool", lambda: nc.gpsimd.memset(onesT[:], 1.0), writes=[onesT])
                    init = [sb(e2, f"s5init{k}", [128, 1]) for k in range(2)]
                    tmpc = sb(e2, "s5tmpc", [128, 1])
                    gt = sb(e2, "s5gt", [128, L])
                    pv1 = [ps(e2, f"s5pv1{k}") for k in range(2)]
                    pv2 = [ps(e2, f"s5pv2{k}") for k in range(2)]
                    pY = [ps(e2, f"s5pY{k}") for k in range(2)]
                    pT = ps(e2, "s5pT")
                    pJ = ps(e2, "s5pJ", [128, 8])
                    iota = ccol("iota")
                    nu = 0
                    for c in range(NCH):
                        x = xc[0]
                        dma("sp", x[:], xT[c, :, :], [DB("x", c)], [x])
                        S.op("dve", (lambda x=x: nc.vector.tensor_tensor(out=u32[:], in0=x[:], in1=rstd[:], op=ALU.mult)),
                             reads=[x, rstd], writes=[u32])
                        S.op("act", (lambda c=c: nc.scalar.activation(out=u32[:], in_=u32[:], func=AF.Identity,
                                                                       scale=acol(i, 0, c), bias=modc(i, 0, c))),
                             reads=[u32, AB, MOD], writes=[u32])
                        S.op("act", lambda: nc.scalar.copy(out=ub[:], in_=u32[:]), reads=[u32], writes=[ub])
                        dump("u32", u32, [128, L])
                        gs = slice(c * 128, (c + 1) * 128)
                        dma("sp", Bri[:], s5B[j, 0, :, c * 128:(c + 1) * 128], [], [Bri])
                        dma("sp", Bir[:], s5B[j, 1, :, c * 128:(c + 1) * 128], [], [Bir])
                        dma("sp", Cst[:], s5C[j, 0, :, c * 128:(c + 1) * 128], [], [Cst])
                        dma("sp", Csw[:], s5C[j, 1, :, c * 128:(c + 1) * 128], [], [Csw])
                        for g in range(8):
                            G = c * 8 + g
                            hs = slice(g * 16, (g + 1) * 16)
                            vop(lambda G=G, hs=hs: nc.vector.tensor_scalar(bt[:], Bir[:, hs], cf["ncis"][:, G:G + 1], None,
                                                                             op0=ALU.mult), [Bir, cf["ncis"]], [bt])
                            vop(lambda G=G, hs=hs: nc.vector.scalar_tensor_tensor(out=B1s[:, hs], in0=Bri[:, hs],
                                                                                    scalar=cf["cr"][:, G:G + 1], in1=bt[:],
                                                                                    op0=ALU.mult, op1=ALU.add),
                                [Bri, cf["cr"], bt], [B1s])
                            vop(lambda G=G, hs=hs: nc.vector.tensor_scalar(bt[:], Bri[:, hs], cf["nci"][:, G:G + 1], None,
                                                                             op0=ALU.mult), [Bri, cf["nci"]], [bt])
                            vop(lambda G=G, hs=hs: nc.vector.scalar_tensor_tensor(out=B2s[:, hs], in0=Bir[:, hs],
                                                                                    scalar=cf["ncrs"][:, G:G + 1], in1=bt[:],
                                                                                    op0=ALU.mult, op1=ALU.add),
                                [Bir, cf["ncrs"], bt], [B2s])
                            vop(lambda g=g, hs=hs: nc.vector.tensor_scalar(CaZ[:, g, hs], Cst[:, hs], sgn, None, op0=ALU.mult),
                                [Cst, C], [CaZ])
                            vop(lambda g=g, hs=hs: nc.vector.tensor_scalar(CbZ[:, g, hs], Csw[:, hs], -1.0, None, op0=ALU.mult),
                                [Csw], [CbZ])
                        for Bs, Bg in ((B1s, B1g), (B2s, B2g)):
                            S.op("pe", (lambda Bs=Bs: nc.tensor.transpose(pT[:, 0:128], Bs[:], ident)), reads=[Bs, C], writes=[pT])
                            fence(pT[:, 0:128], 128, 128, [], [pT])
                            for g in range(8):
                                vop(lambda Bg=Bg, g=g: nc.vector.tensor_scalar(Bg[:, g, :], pT[:, 0:128], ccol("gmask", g, 1),
                                                                                 None, op0=ALU.mult), [pT, C], [Bg])
                        for g in range(8):
                            G = c * 8 + g
                            tb = tabs[G % 2]
                            wtl = [th, tq0, tq1, snt, cst_]
                            vop(lambda G=G: nc.vector.tensor_scalar(th[:], iota, cf["wi"][:, G:G + 1], None, op0=ALU.mult),
                                [C, cf["wi"]], [th])
                            sincos(th[:], snt[:], cst_[:], wtl, wtl, tq0[:], tq1[:])
                            S.op("act", (lambda G=G: nc.scalar.activation(out=mgF[:], in_=iota, func=AF.Exp,
                                                                           scale=cf["wr"][:, G:G + 1])),
                                 reads=[C, cf["wr"]], writes=[mgF])
                            S.op("act", (lambda G=G: nc.scalar.activation(out=mgE[:], in_=iota, func=AF.Exp,
                                                                           scale=nwr[:, G:G + 1])),
                                 reads=[C, nwr], writes=[mgE])
                            S.op("dve", (lambda tb=tb: nc.vector.tensor_tensor(out=tb["Ere"][:], in0=cst_[:], in1=mgE[:],
                                                                                 op=ALU.mult)), reads=[cst_, mgE], writes=[tb["Ere"]])
                            S.op("dve", (lambda tb=tb: nc.vector.tensor_tensor(out=tb["Eim"][:], in0=snt[:], in1=mgE[:],
                                                                                 op=ALU.mult)), reads=[snt, mgE], writes=[tb["Eim"]])
                            S.op("dve", (lambda tb=tb: nc.vector.tensor_tensor(out=tb["Fre"][:], in0=cst_[:], in1=mgF[:],
                                                                                 op=ALU.mult)), reads=[cst_, mgF], writes=[tb["Fre"]])
                            S.op("dve", (lambda tb=tb: nc.vector.tensor_tensor(out=tb["Fim"][:], in0=snt[:], in1=mgF[:],
                                                                                 op=ALU.mult)), reads=[snt, mgF], writes=[tb["Fim"]])
                            ini = init[0]
                            S.op("dve", (lambda ini=ini: nc.vector.memset(ini[:], 0.0)), writes=[ini])
                            for k_ in ("Ere", "Eim", "Fre", "Fim"):
                                dump("tab_" + k_, tb[k_], [128, TB + 1])
                            dump("B1s", B1s, [128, 128]); dump("B2s", B2s, [128, 128])
                            for blk in range(NB):
                                k = nu % 2
                                nu += 1
                                bs = slice(blk * TB, (blk + 1) * TB)
                                S.op("pe", (lambda k=k, g=g, bs=bs: nc.tensor.matmul(pv1[k][:], lhsT=B1g[:, g, :], rhs=ub[:, bs],
                                                                                      start=True, stop=True)),
                                     reads=[B1g, ub], writes=[pv1[k]])
                                S.op("pe", (lambda k=k, g=g, bs=bs: nc.tensor.matmul(pv2[k][:], lhsT=B2g[:, g, :], rhs=ub[:, bs],
                                                                                      start=True, stop=True)),
                                     reads=[B2g, ub], writes=[pv2[k]])
                                vop(lambda k=k, tb=tb: nc.vector.tensor_tensor(out=W1[k][:], in0=pv1[k][:], in1=tb["Ere"][:, 0:TB],
                                                                               op=ALU.mult), [pv1[k], tb["Ere"]], [W1[k]])
                                vop(lambda k=k, tb=tb: nc.vector.tensor_tensor(out=W2[k][:], in0=pv2[k][:], in1=tb["Eim"][:, 0:TB],
                                                                               op=ALU.mult), [pv2[k], tb["Eim"]], [W2[k]])
                                cur_ini = init[blk % 2]
                                vop(lambda k=k, cur_ini=cur_ini: nc.vector.tensor_tensor_scan(
                                    out=Pp[k][:], data0=W2[k][:], data1=W1[k][:], initial=cur_ini[:, 0:1], op0=ALU.add,
                                    op1=ALU.add), [W2[k], W1[k], cur_ini], [Pp[k]])
                                if blk + 1 < NB:
                                    nxt = init[(blk + 1) % 2]
                                    S.op("pe", (lambda k=k: nc.tensor.matmul(pJ[:, 0:1], lhsT=ccol("J"), rhs=Pp[k][:, TB - 1:TB],
                                                                             start=True, stop=True)),
                                         reads=[Pp[k], C], writes=[pJ])
                                    fence(pJ[:, 0:1], 128, 1, [], [pJ])
                                    vop(lambda tb=tb: nc.vector.tensor_tensor(out=tmpc[:], in0=pJ[:, 0:1], in1=tb["Fim"][:, TB:TB + 1],
                                                                              op=ALU.mult), [pJ, tb["Fim"]], [tmpc])
                                    vop(lambda k=k, tb=tb, nxt=nxt: nc.vector.scalar_tensor_tensor(
                                        out=nxt[:], in0=Pp[k][:, TB - 1:TB], scalar=tb["Fre"][:, TB:TB + 1], in1=tmpc[:],
                                        op0=ALU.mult, op1=ALU.add), [Pp[k], tb["Fre"], tmpc], [nxt])
                                S.op("dve", (lambda k=k, tb=tb: nc.vector.tensor_tensor(out=Q1[k][:], in0=Pp[k][:],
                                                                                          in1=tb["Fre"][:, 0:TB], op=ALU.mult)),
                                     reads=[Pp[k], tb["Fre"]], writes=[Q1[k]])
                                vop(lambda k=k, tb=tb: nc.vector.tensor_tensor(out=Q2[k][:], in0=Pp[k][:], in1=tb["Fim"][:, 0:TB],
                                                                               op=ALU.mult), [Pp[k], tb["Fim"]], [Q2[k]])
                                S.op("pe", (lambda k=k, g=g: nc.tensor.matmul(pY[k][:], lhsT=CaZ[:, g, :], rhs=Q1[k][:],
                                                                              start=True, stop=False)),
                                     reads=[CaZ, Q1[k]], writes=[pY[k]])
                                S.op("pe", (lambda k=k, g=g: nc.tensor.matmul(pY[k][:], lhsT=CbZ[:, g, :], rhs=Q2[k][:],
                                                                              start=False, stop=True)),
                                     reads=[CbZ, Q2[k]], writes=[pY[k]])
                                if g == 0:
                                    vop(lambda k=k, bs=bs, c=c: nc.vector.scalar_tensor_tensor(
                                        out=yacc[:, bs], in0=u32[:, bs], scalar=vcol("s5_d", j * NCH + c), in1=pY[k][:],
                                        op0=ALU.mult, op1=ALU.add), [u32, V, pY[k]], [yacc])
                                else:
                                    vop(lambda k=k, bs=bs: nc.vector.tensor_tensor(out=yacc[:, bs], in0=yacc[:, bs], in1=pY[k][:],
                                                                                   op=ALU.add), [yacc, pY[k]], [yacc])
                        dump("yacc", yacc, [128, L])
                        z = zb[0]
                        S.op("dve", lambda: nc.vector.tensor_tensor(out=gt[:], in0=yacc[:], in1=yacc[:], op=ALU.mult),
                             reads=[yacc], writes=[gt])
                        S.op("dve", lambda: nc.vector.tensor_scalar(gt[:], gt[:], 0.044715, 1.0, op0=ALU.mult, op1=ALU.add),
                             reads=[gt], writes=[gt])
                        S.op("dve", lambda: nc.vector.tensor_tensor(out=gt[:], in0=gt[:], in1=yacc[:], op=ALU.mult),
                             reads=[gt, yacc], writes=[gt])
                        S.op("act", lambda: nc.scalar.activation(out=gt[:], in_=gt[:], func=AF.Sigmoid, scale=1.5957691216),
                             reads=[gt], writes=[gt])
                        S.op("dve", (lambda z=z: nc.vector.tensor_tensor(out=z[:], in0=gt[:], in1=yacc[:], op=ALU.mult)),
                             reads=[gt, yacc], writes=[z])
                        dma("sp", actT[c, :, :], z[:], [z], [DB("act", c)])
            proj_phase(i, w_glu[j], 2 * D, glu=True)

        def proj_phase(i, wsrc, ncols, glu):
            with scope() as e1:
                A = sb(e1, "pjA", [128, NCH, L], BF16)
                for c in range(NCH):
                    dma("sp", A[:, c, :], actT[c, :, :], [DB("act", c)], [A])
                nW = 2 if glu else 1
                Wt = [[sb(e1, f"pjW{k}_{q}", [128, NCH, 128], BF16) for q in range(nW)] for k in range(2)]
                xt = [sb(e1, f"pjx{k}", [128, TB]) for k in range(3)]
                sg = [sb(e1, f"pjs{k}", [128, TB]) for k in range(2)]
                pa = [ps(e1, f"pjpa{k}") for k in range(2)]
                pg = [ps(e1, f"pjpg{k}") for k in range(2)]
                n = 0
                def load_w(jc):
                    for q in range(nW):
                        src = wsrc[:, q * D + jc * 128: q * D + (jc + 1) * 128].rearrange("(k p) n -> p k n", p=128)
                        dma("pool", Wt[jc % 2][q][:], src, [], [Wt[jc % 2][q]])

                load_w(0)
                for jc in range(NCH):
                    W = Wt[jc % 2]
                    if jc + 1 < NCH:
                        load_w(jc + 1)
                    for blk in range(NB):
                        bs = slice(blk * TB, (blk + 1) * TB)
                        k2 = n % 2
                        x = xt[n % 3]
                        n += 1
                        dma("sp", x[:], xT[jc, :, bs], [DB("x", jc)], [x])
                        for k in range(NCH):
                            S.op("pe", (lambda W=W, k=k, bs=bs, k2=k2: nc.tensor.matmul(
                                pa[k2][:], lhsT=W[0][:, k, :], rhs=A[:, k, bs], start=(k == 0), stop=(k == NCH - 1))),
                                 reads=[W[0], A], writes=[pa[k2]])
                        if glu:
                            for k in range(NCH):
                                S.op("pe", (lambda W=W, k=k, bs=bs, k2=k2: nc.tensor.matmul(
                                    pg[k2][:], lhsT=W[1][:, k, :], rhs=A[:, k, bs], start=(k == 0), stop=(k == NCH - 1))),
                                     reads=[W[1], A], writes=[pg[k2]])
                            S.op("act", (lambda k2=k2: nc.scalar.activation(out=sg[k2][:], in_=pg[k2][:], func=AF.Sigmoid)),
                                 reads=[pg[k2]], writes=[sg[k2]])
                            S.op("dve", (lambda k2=k2: nc.vector.tensor_tensor(out=sg[k2][:], in0=pa[k2][:], in1=sg[k2][:],
                                                                               op=ALU.mult)), reads=[pa[k2], sg[k2]], writes=[sg[k2]])
                            S.op("dve", (lambda k2=k2, x=x, jc=jc: nc.vector.scalar_tensor_tensor(
                                out=x[:], in0=sg[k2][:], scalar=modc(i, 2, jc), in1=x[:], op0=ALU.mult, op1=ALU.add)),
                                 reads=[sg[k2], MOD, x], writes=[x])
                        else:
                            S.op("dve", (lambda k2=k2, x=x, jc=jc: nc.vector.scalar_tensor_tensor(
                                out=x[:], in0=pa[k2][:], scalar=modc(i, 2, jc), in1=x[:], op0=ALU.mult, op1=ALU.add)),
                                 reads=[pa[k2], MOD, x], writes=[x])
                        dma("sp", xT[jc, :, bs], x[:], [x], [DB("x", jc)])

        def hg_layer(i):
            j = i // 2
            with scope() as e1:
                rstd = rstd_phase(e1, xT)
                with scope() as e2:
                    xc = [sb(e2, f"hgx{k}", [128, L]) for k in range(2)]
                    hb_ = [sb(e2, f"hgh{k}", [128, L], BF16) for k in range(2)]
                    for c in range(NCH):
                        x = xc[c % 2]
                        hb = hb_[c % 2]
                        dma("sp", x[:], xT[c, :, :], [DB("x", c)], [x])
                        S.op("dve", (lambda x=x: nc.vector.tensor_tensor(out=x[:], in0=x[:], in1=rstd[:], op=ALU.mult)),
                             reads=[x, rstd], writes=[x])
                        S.op("act", (lambda x=x, hb=hb, c=c: nc.scalar.activation(out=hb[:], in_=x[:], func=AF.Identity,
                                                                                   scale=acol(i, 0, c), bias=modc(i, 0, c))),
                             reads=[x, AB, MOD], writes=[hb])
                        dma("sp", hT[c, :, :], hb[:], [hb], [DB("h", c)])
            with scope() as e1:
                Wt = [[sb(e1, f"hgW{k}_{q}", [128, NCH, 128], BF16) for q in range(4)] for k in range(2)]
                hbk = [sb(e1, f"hghb{k}", [128, NCH, TB], BF16) for k in range(2)]
                S32 = [sb(e1, f"hgS32{k}", [128, 128]) for k in range(2)]
                Sbf = [sb(e1, f"hgSbf{k}", [128, 128], BF16) for k in range(2)]
                f32t = {k: sb(e1, "hg_" + k, [128, TB]) for k in ("sig", "f", "lf", "k", "b", "eb", "enb", "kk", "gs", "o", "sq", "r")}
                qt = sb(e1, "hg_qt", [128, TB], BF16)
                kt = sb(e1, "hg_kt", [128, TB], BF16)
                kh = sb(e1, "hg_kh", [128, TB], BF16)
                vtm = sb(e1, "hg_vtm", [128, 4, 128], BF16)
                khT = [sb(e1, f"hg_khT{k}", [128, 128], BF16) for k in range(2)]
                smk = [sb(e1, f"hg_sm{k}", [128, 128], BF16) for k in range(2)]
                onb = [sb(e1, f"hg_onb{k}", [128, TB], BF16) for k in range(2)]
                pq = ps(e1, "hgpq")
                pf = ps(e1, "hgpf")
                pgt = ps(e1, "hgpg")
                pv = ps(e1, "hgpv")
                pss = [ps(e1, f"hgps{k}", [128, 128]) for k in range(1)]
                pso = ps(e1, "hgpo", [128, 128])
                pst = ps(e1, "hgpt", [128, 128], BF16)
                pS = ps(e1, "hgpS", [128, 128])
                cmask = ccol("cmask")
                bcmask = ccol("bcmask")
                nb = 0
                def load_head(hd):
                    for q in range(4):
                        src = w_in[j][:, q * D + hd * 128: q * D + (hd + 1) * 128].rearrange("(k p) n -> p k n", p=128)
                        dma("pool", Wt[hd % 2][q][:], src, [], [Wt[hd % 2][q]])

                load_head(0)
                for hd in range(NCH):
                    W = Wt[hd % 2]
                    if hd + 1 < NCH:
                        load_head(hd + 1)
                    S.op("dve", lambda: nc.vector.memset(S32[0][:], 0.0), writes=[S32[0]])
                    S.op("dve", lambda: nc.vector.memset(Sbf[0][:], 0.0), writes=[Sbf[0]])
                    sidx = 0
                    lbc = LB[:, i * NCH + hd: i * NCH + hd + 1]
                    omlc = LB[:, DEPTH * NCH + i * NCH + hd: DEPTH * NCH + i * NCH + hd + 1]
                    for blk in range(NB):
                        bs = slice(blk * TB, (blk + 1) * TB)
                        hb = hbk[nb % 2]
                        ob = onb[nb % 2]
                        nb += 1
                        dma("sp", hb[:], hT[:, :, bs].rearrange("k p t -> p k t"), [DB("h", c) for c in range(NCH)], [hb])
                        for q, pp in ((0, pq), (1, pf), (3, pgt)):
                            for k in range(NCH):
                                S.op("pe", (lambda W=W, q=q, pp=pp, k=k, hb=hb: nc.tensor.matmul(
                                    pp[:], lhsT=W[q][:, k, :], rhs=hb[:, k, :], start=(k == 0), stop=(k == NCH - 1))),
                                     reads=[W[q], hb], writes=[pp])
                        for ts in range(4):
                            for k in range(NCH):
                                S.op("pe", (lambda W=W, ts=ts, k=k, hb=hb: nc.tensor.matmul(
                                    pv[:, ts * 128:(ts + 1) * 128], lhsT=hb[:, k, ts * 128:(ts + 1) * 128], rhs=W[2][:, k, :],
                                    start=(k == 0), stop=(k == NCH - 1))), reads=[W[2], hb], writes=[pv])
                        F = f32t
                        S.op("act", lambda: nc.scalar.copy(out=vtm[:].rearrange("p a b -> p (a b)"), in_=pv[:]), reads=[pv], writes=[vtm])
                        S.op("act", lambda: nc.scalar.activation(out=F["sig"][:], in_=pf[:], func=AF.Sigmoid), reads=[pf], writes=[F["sig"]])
                        S.op("dve", (lambda omlc=omlc, lbc=lbc: nc.vector.tensor_scalar(F["f"][:], F["sig"][:], omlc, lbc, op0=ALU.mult,
                                                                                         op1=ALU.add)), reads=[F["sig"], LB], writes=[F["f"]])
                        S.op("act", lambda: nc.scalar.activation(out=F["lf"][:], in_=F["f"][:], func=AF.Ln), reads=[F["f"]], writes=[F["lf"]])
                        S.op("dve", lambda: nc.vector.tensor_scalar(F["k"][:], F["f"][:], -1.0, 1.0, op0=ALU.mult, op1=ALU.add),
                             reads=[F["f"]], writes=[F["k"]])
                        S.op("dve", lambda: nc.vector.tensor_tensor_scan(out=F["b"][:], data0=cmask, data1=F["lf"][:], initial=0.0,
                                                                         op0=ALU.mult, op1=ALU.add), reads=[C, F["lf"]], writes=[F["b"]])
                        S.op("act", lambda: nc.scalar.activation(out=F["eb"][:], in_=F["b"][:], func=AF.Exp), reads=[F["b"]], writes=[F["eb"]])
                        S.op("act", lambda: nc.scalar.activation(out=F["enb"][:], in_=F["b"][:], func=AF.Exp, scale=-1.0),
                             reads=[F["b"]], writes=[F["enb"]])
                        S.op("dve", lambda: nc.vector.tensor_tensor(out=qt[:], in0=pq[:], in1=F["eb"][:], op=ALU.mult),
                             reads=[pq, F["eb"]], writes=[qt])
                        S.op("dve", lambda: nc.vector.tensor_tensor(out=F["kk"][:], in0=F["k"][:], in1=F["enb"][:], op=ALU.mult),
                             reads=[F["k"], F["enb"]], writes=[F["kk"]])
                        S.op("act", lambda: nc.scalar.copy(out=kt[:], in_=F["kk"][:]), reads=[F["kk"]], writes=[kt])
                        for ch in range(8):
                            cs_ = slice(ch * 64, (ch + 1) * 64)
                            S.op("dve", (lambda cs_=cs_, ch=ch: nc.vector.tensor_scalar(kh[:, cs_], F["kk"][:, cs_],
                                                                                         F["eb"][:, ch * 64 + 63: ch * 64 + 64], None,
                                                                                         op0=ALU.mult)), reads=[F["kk"], F["eb"]], writes=[kh])
                        S.op("act", lambda: nc.scalar.activation(out=F["gs"][:], in_=pgt[:], func=AF.Silu), reads=[pgt], writes=[F["gs"]])
                        for ts in range(4):
                            t0 = ts * 128
                            kT_ = khT[ts % 2]
                            sm_ = smk[ts % 2]
                            S.op("pe", (lambda t0=t0: nc.tensor.transpose(pst[:, 0:128], kh[:, t0:t0 + 128], ident_bf[:])), reads=[kh, ident_bf],
                                 writes=[pst])
                            S.op("act", (lambda kT_=kT_: nc.scalar.copy(out=kT_[:], in_=pst[:, 0:128])), reads=[pst], writes=[kT_])
                            S.op("pe", (lambda t0=t0: nc.tensor.matmul(pss[0][:, 0:128], lhsT=kt[:, t0:t0 + 128], rhs=qt[:, t0:t0 + 128],
                                                                      start=True, stop=True)), reads=[kt, qt], writes=[pss[0]])
                            S.op("dve", (lambda sm_=sm_: nc.vector.tensor_tensor(out=sm_[:], in0=pss[0][:, 0:128], in1=bcmask, op=ALU.mult)),
                                 reads=[pss[0], C], writes=[sm_])
                            S.op("pe", (lambda ts=ts, sm_=sm_: nc.tensor.matmul(pso[:, 0:128], lhsT=vtm[:, ts, :], rhs=sm_[:], start=True,
                                                                               stop=False)), reads=[vtm, sm_], writes=[pso])
                            for half in range(2):
                                hs = slice(half * 64, (half + 1) * 64)
                                sa = sidx % 2
                                sn_ = (sidx + 1) % 2
                                sidx += 1
                                S.op("pe", (lambda sa=sa, t0=t0, hs=hs, half=half: nc.tensor.matmul(
                                    pso[:, hs], lhsT=Sbf[sa][:], rhs=qt[:, t0 + half * 64: t0 + half * 64 + 64], start=False,
                                    stop=(half == 1))), reads=[Sbf[sa], qt], writes=[pso])
                                S.op("pe", (lambda kT_=kT_, hs=hs, ts=ts: nc.tensor.matmul(
                                    pS[:, 0:128], lhsT=kT_[hs, :], rhs=vtm[hs, ts, :], start=True, stop=True)), reads=[kT_, vtm], writes=[pS])
                                ec = F["eb"][:, t0 + half * 64 + 63: t0 + half * 64 + 64]
                                S.op("dve", (lambda sa=sa, sn_=sn_, ec=ec: nc.vector.scalar_tensor_tensor(
                                    out=S32[sn_][:], in0=S32[sa][:], scalar=ec, in1=pS[:, 0:128], op0=ALU.mult, op1=ALU.add)),
                                     reads=[S32[sa], F["eb"], pS], writes=[S32[sn_]])
                                S.op("act", (lambda sn_=sn_: nc.scalar.copy(out=Sbf[sn_][:], in_=S32[sn_][:])), reads=[S32[sn_]],
                                     writes=[Sbf[sn_]])
                            S.op("act", (lambda t0=t0: nc.scalar.copy(out=F["o"][:, t0:t0 + 128], in_=pso[:, 0:128])), reads=[pso], writes=[F["o"]])
                        if sidx % 2 == 1:
                            raise AssertionError
                        S.op("act", lambda: nc.scalar.activation(out=F["sq"][:], in_=F["o"][:], func=AF.Square), reads=[F["o"]], writes=[F["sq"]])
                        S.op("pe", lambda: nc.tensor.matmul(pq[:], lhsT=onesD[:], rhs=F["sq"][:], start=True, stop=True),
                             reads=[onesD, F["sq"]], writes=[pq])
                        fence(pq[:], 128, TB, [], [pq])
                        S.op("act", lambda: nc.scalar.activation(out=F["r"][:], in_=pq[:], func=AF.Sqrt, bias=epsc[:, 0:1]),
                             reads=[pq, epsc], writes=[F["r"]])
                        S.op("dve", lambda: nc.vector.reciprocal(out=F["r"][:], in_=F["r"][:]), reads=[F["r"]], writes=[F["r"]])
                        S.op("dve", lambda: nc.vector.tensor_tensor(out=F["o"][:], in0=F["o"][:], in1=F["r"][:], op=ALU.mult),
                             reads=[F["o"], F["r"]], writes=[F["o"]])
                        S.op("dve", lambda: nc.vector.tensor_tensor(out=F["o"][:], in0=F["o"][:], in1=F["gs"][:], op=ALU.mult),
                             reads=[F["o"], F["gs"]], writes=[F["o"]])
                        S.op("act", (lambda ob=ob: nc.scalar.activation(out=ob[:], in_=F["o"][:], func=AF.Identity,
                                                                         scale=vcol("hg_norm", j))), reads=[F["o"], V], writes=[ob])
                        dma("sp", actT[hd, :, bs], ob[:], [ob], [DB("act", hd)])
            proj_phase(i, w_out[j], D, glu=False)

        ident_bf = sb(es, "ident_bf", [128, 128], BF16)
        S.op("dve", lambda: nc.vector.tensor_copy(out=ident_bf[:], in_=ident), reads=[C], writes=[ident_bf])

        def moe_layer(i):
            with scope() as e1:
                GT = sb(e1, "moeGT", [NE, L])
                with scope() as e2:
                    rstd = rstd_phase(e2, xT)
                    xt = [sb(e2, f"mx{k}", [128, TB]) for k in range(3)]
                    h2f = sb(e2, "mh2f", [128, NCH, TB])
                    h2b = [sb(e2, f"mh2b{k}", [128, NCH, TB], BF16) for k in range(2)]
                    wr = sb(e2, "mwr", [128, NCH, NE])
                    rb = sb(e2, "mrb", [1, DEPTH * NE])
                    lg = sb(e2, "mlg", [128, NE])
                    t8 = sb(e2, "mt8", [128, 8])
                    mk = sb(e2, "mmk", [128, NE])
                    ex = sb(e2, "mex", [128, NE])
                    sc = sb(e2, "msc", [128, 4])
                    pl = ps(e2, "mpl", [128, NE])
                    pt = ps(e2, "mpt", [NE, 128])
                    dma("sp", wr[:], router_w[i].rearrange("(k p) e -> p k e", p=128), [], [wr])
                    dma("sp", rb[:], router_b[:, :], [], [rb])
                    n = 0
                    for blk in range(NB):
                        bs = slice(blk * TB, (blk + 1) * TB)
                        hbb = h2b[blk % 2]
                        for c in range(NCH):
                            x = xt[n % 3]
                            n += 1
                            dma("sp", x[:], xT[c, :, bs], [DB("x", c)], [x])
                            S.op("dve", (lambda x=x, bs=bs: nc.vector.tensor_tensor(out=x[:], in0=x[:], in1=rstd[:, bs], op=ALU.mult)),
                                 reads=[x, rstd], writes=[x])
                            S.op("act", (lambda x=x, c=c: nc.scalar.activation(out=h2f[:, c, :], in_=x[:], func=AF.Identity,
                                                                                scale=acol(i, 1, c), bias=modc(i, 3, c))),
                                 reads=[x, AB, MOD], writes=[h2f])
                        S.op("act", (lambda hbb=hbb: nc.scalar.copy(out=hbb[:], in_=h2f[:])), reads=[h2f], writes=[hbb])
                        dma("sp", hT[:, :, bs].rearrange("k p t -> p k t"), hbb[:], [hbb], [DB("h", c) for c in range(NCH)])
                        for ts in range(4):
                            t0 = ts * 128
                            for k in range(NCH):
                                S.op("pe", (lambda k=k, t0=t0: nc.tensor.matmul(pl[:, 0:NE], lhsT=h2f[:, k, t0:t0 + 128], rhs=wr[:, k, :],
                                                                                start=(k == 0), stop=False)), reads=[h2f, wr], writes=[pl])
                            S.op("pe", lambda: nc.tensor.matmul(pl[:, 0:NE], lhsT=ones[0:1, :], rhs=rb[0:1, i * NE:(i + 1) * NE], start=False,
                                                                stop=True), reads=[C, rb], writes=[pl])
                            fence(pl[:, 0:NE], 128, NE, [], [pl])
                            S.op("act", lambda: nc.scalar.copy(out=lg[:], in_=pl[:, 0:NE]), reads=[pl], writes=[lg])
                            S.op("dve", lambda: nc.vector.max(out=t8[:], in_=lg[:]), reads=[lg], writes=[t8])
                            S.op("dve", lambda: nc.vector.tensor_scalar(mk[:], lg[:], t8[:, 3:4], None, op0=ALU.is_ge), reads=[lg, t8], writes=[mk])
                            S.op("dve", lambda: nc.vector.tensor_scalar(sc[:, 0:1], t8[:, 0:1], -1.0, None, op0=ALU.mult), reads=[t8], writes=[sc])
                            S.op("act", lambda: nc.scalar.activation(out=ex[:], in_=lg[:], func=AF.Exp, bias=sc[:, 0:1]), reads=[lg, sc], writes=[ex])
                            S.op("dve", lambda: nc.vector.tensor_tensor(out=ex[:], in0=ex[:], in1=mk[:], op=ALU.mult), reads=[ex, mk], writes=[ex])
                            S.op("dve", lambda: nc.vector.reduce_sum(out=sc[:, 1:2], in_=ex[:], axis=AX.X), reads=[ex], writes=[sc])
                            S.op("dve", lambda: nc.vector.reciprocal(out=sc[:, 2:3], in_=sc[:, 1:2]), reads=[sc], writes=[sc])
                            S.op("dve", lambda: nc.vector.tensor_scalar(ex[:], ex[:], sc[:, 2:3], None, op0=ALU.mult), reads=[ex, sc], writes=[ex])
                            S.op("pe", lambda: nc.tensor.matmul(pt[0:NE, 0:128], lhsT=ex[:], rhs=ident, start=True, stop=True), reads=[ex, C], writes=[pt])
                            fence(pt[0:NE, 0:128], NE, 128, [], [pt])
                            tok = slice(blk * TB + t0, blk * TB + t0 + 128)
                            S.op("act", (lambda tok=tok: nc.scalar.copy(out=GT[:, tok], in_=pt[0:NE, 0:128])), reads=[pt], writes=[GT])
                Wg = [sb(e1, f"moWg{k}", [128, NCH, 2 * FF], BF16) for k in range(2)]
                Wd = [sb(e1, f"moWd{k}", [128, 4, D], BF16) for k in range(2)]
                hbk = [sb(e1, f"mohb{k}", [128, NCH, TB], BF16) for k in range(2)]
                act = [sb(e1, f"moact{k}", [128, 4, TB], BF16) for k in range(2)]
                sels = [sb(e1, f"mosel{k}", [NE, 128]) for k in range(2)]
                BU1 = sb(e1, "moBU1", [128, NE * 8])
                S.op("dve", lambda: nc.vector.tensor_scalar(BU1[:], vcol("b_gu", i * NE * 8, NE * 8), 1.0, None, op0=ALU.add),
                     reads=[V], writes=[BU1])
                gtt = [sb(e1, f"mogt{k}", [128, TB]) for k in range(2)]
                sgt = [sb(e1, f"mosg{k}", [128, TB]) for k in range(2)]
                upt = [sb(e1, f"moup{k}", [128, TB]) for k in range(2)]
                yt = [sb(e1, f"moy{k}", [128, TB]) for k in range(4)]
                pG = [ps(e1, f"mopG{k}") for k in range(2)]
                pU = [ps(e1, f"mopU{k}") for k in range(2)]
                pB = ps(e1, "mopB")
                pY = [ps(e1, f"mopY{k}") for k in range(3)]
                nb = 0
                nj = 0
                ny = 0
                with scope() as e2:
                    bd = sb(e2, "mobd", [NE, D])
                    dma("sp", bd[:], b_dn[LI[i]], [], [bd])
                    for dc in range(NCH):
                        for blk in range(NB):
                            bs = slice(blk * TB, (blk + 1) * TB)
                            py = pY[ny % 3]
                            y = yt[ny % 4]
                            ny += 1
                            S.op("pe", (lambda dc=dc, py=py, bs=bs: nc.tensor.matmul(py[:], lhsT=bd[:, dc * 128:(dc + 1) * 128], rhs=GT[:, bs],
                                                                                    start=True, stop=True)), reads=[bd, GT], writes=[py])
                            fence(py[:], 128, TB, [], [py])
                            S.op("act", (lambda y=y, py=py: nc.scalar.copy(out=y[:], in_=py[:])), reads=[py], writes=[y])
                            dma("sp", ymoe[dc, :, bs], y[:], [y], [DB("ym", dc, blk)])
                def load_expert(e):
                    wg_, wd_ = Wg[e % 2], Wd[e % 2]
                    for q in range(4):
                        dma("pool", wg_[:, 4 * q:4 * q + 4, :],
                            w_gu[LI[i], e, q * 512:(q + 1) * 512, :].rearrange("(k p) n -> p k n", p=128), [], [wg_])
                    dma("pool", wd_[:], w_dn[LI[i], e].rearrange("(k p) n -> p k n", p=128), [], [wd_])

                load_expert(0)
                for e in range(NE):
                    wg = Wg[e % 2]
                    wd = Wd[e % 2]
                    sel = sels[e % 2]
                    S.op("dve", (lambda e=e, sel=sel: nc.vector.tensor_scalar(sel[:], ones[0:NE, :], ident[0:NE, e:e + 1], None,
                                                                             op0=ALU.mult)), reads=[C], writes=[sel])
                    for blk in range(NB):
                        bs = slice(blk * TB, (blk + 1) * TB)
                        hb = hbk[nb % 2]
                        ac = act[nb % 2]
                        nb += 1
                        dma("sp", hb[:], hT[:, :, bs].rearrange("k p t -> p k t"), [DB("h", c) for c in range(NCH)], [hb])
                        if blk == min(1, NB - 1) and e + 1 < NE:
                            load_expert(e + 1)
                        S.op("pe", (lambda sel=sel, bs=bs: nc.tensor.matmul(pB[:], lhsT=sel[:], rhs=GT[:, bs], start=True,
                                                                      stop=True)), reads=[sel, GT], writes=[pB])
                        fence(pB[:], 128, TB, [], [pB])
                        for jq in range(4):
                            k2 = nj % 2
                            nj += 1
                            for k in range(NCH):
                                S.op("pe", (lambda wg=wg, k=k, jq=jq, hb=hb, k2=k2: nc.tensor.matmul(
                                    pG[k2][:], lhsT=wg[:, k, jq * 128:(jq + 1) * 128], rhs=hb[:, k, :], start=(k == 0), stop=(k == NCH - 1))),
                                     reads=[wg, hb], writes=[pG[k2]])
                            for k in range(NCH):
                                S.op("pe", (lambda wg=wg, k=k, jq=jq, hb=hb, k2=k2: nc.tensor.matmul(
                                    pU[k2][:], lhsT=wg[:, k, FF + jq * 128: FF + (jq + 1) * 128], rhs=hb[:, k, :], start=(k == 0),
                                    stop=(k == NCH - 1))), reads=[wg, hb], writes=[pU[k2]])
                            bgc = vcol("b_gu", (i * NE + e) * 8 + jq)
                            buc = vcol("b_gu", (i * NE + e) * 8 + 4 + jq)
                            g_, s_, u_ = gtt[k2], sgt[k2], upt[k2]
                            bu1c = BU1[:, e * 8 + 4 + jq: e * 8 + 4 + jq + 1]
                            S.op("dve", (lambda g_=g_, k2=k2, bgc=bgc: nc.vector.tensor_scalar(g_[:], pG[k2][:], bgc, 7.0, op0=ALU.add, op1=ALU.min)),
                                 reads=[pG[k2], V], writes=[g_])
                            S.op("act", (lambda g_=g_, s_=s_: nc.scalar.activation(out=s_[:], in_=g_[:], func=AF.Sigmoid, scale=1.702)),
                                 reads=[g_], writes=[s_])
                            S.op("dve", (lambda u_=u_, k2=k2, bu1c=bu1c: nc.vector.tensor_scalar(u_[:], pU[k2][:], bu1c, 8.0, op0=ALU.add, op1=ALU.min)),
                                 reads=[pU[k2], BU1], writes=[u_])
                            S.op("dve", (lambda g_=g_, s_=s_: nc.vector.tensor_tensor(out=g_[:], in0=g_[:], in1=s_[:], op=ALU.mult)),
                                 reads=[g_, s_], writes=[g_])
                            S.op("dve", (lambda g_=g_, u_=u_: nc.vector.scalar_tensor_tensor(out=g_[:], in0=u_[:], scalar=-6.0, in1=g_[:],
                                                                                             op0=ALU.max, op1=ALU.mult)),
                                 reads=[g_, u_], writes=[g_])
                            S.op("dve", (lambda g_=g_, ac=ac, jq=jq: nc.vector.tensor_tensor(out=ac[:, jq, :], in0=g_[:], in1=pB[:], op=ALU.mult)),
                                 reads=[g_, pB], writes=[ac])
                        for dc in range(NCH):
                            py = pY[ny % 3]
                            y = yt[ny % 4]
                            ny += 1
                            for jq in range(4):
                                S.op("pe", (lambda wd=wd, jq=jq, dc=dc, ac=ac, py=py: nc.tensor.matmul(
                                    py[:], lhsT=wd[:, jq, dc * 128:(dc + 1) * 128], rhs=ac[:, jq, :], start=(jq == 0),
                                    stop=(jq == 3))), reads=[wd, ac], writes=[py])
                            S.op("act", (lambda y=y, py=py: nc.scalar.copy(out=y[:], in_=py[:])), reads=[py], writes=[y])
                            dma("pool", ymoe[dc, :, bs], y[:], [y], [DB("ym", dc, blk)], accum_op=ALU.add)
                xt = [sb(e1, f"morx{k}", [128, TB]) for k in range(3)]
                yy = [sb(e1, f"mory{k}", [128, TB]) for k in range(3)]
                n = 0
                for c in range(NCH):
                    for blk in range(NB):
                        bs = slice(blk * TB, (blk + 1) * TB)
                        x = xt[n % 3]
                        y = yy[n % 3]
                        n += 1
                        dma("sp", x[:], xT[c, :, bs], [DB("x", c)], [x])
                        dma("sp", y[:], ymoe[c, :, bs], [DB("ym", c, blk)], [y])
                        S.op("dve", (lambda x=x, y=y, c=c: nc.vector.scalar_tensor_tensor(out=x[:], in0=y[:], scalar=modc(i, 5, c), in1=x[:],
                                                                                          op0=ALU.mult, op1=ALU.add)), reads=[x, y, MOD], writes=[x])
                        dma("sp", xT[c, :, bs], x[:], [x], [DB("x", c)])

        S.flush()
        for t_ in FREEZE + [epsc, ident_bf]:
            t_.b.frozen = True
            t_.b.r = []
        for i in layers:
            if i % 2 == 0:
                s5_layer(i)
            else:
                hg_layer(i)
            if "nomoe" not in dbg:
                moe_layer(i)

        with scope() as e1:
            rstd = rstd_phase(e1, xT)
            xt = [sb(e1, f"fx{k}", [128, TB]) for k in range(3)]
            n = 0
            for c in range(NCH):
                for blk in range(NB):
                    bs = slice(blk * TB, (blk + 1) * TB)
                    x = xt[n % 3]
                    n += 1
                    dma("sp", x[:], xT[c, :, bs], [DB("x", c)], [x])
                    S.op("dve", (lambda x=x, bs=bs, c=c: nc.vector.scalar_tensor_tensor(out=x[:], in0=x[:], scalar=vcol("norm_final", c),
                                                                                        in1=rstd[:, bs], op0=ALU.mult, op1=ALU.mult)),
                         reads=[x, rstd, V], writes=[x])
                    dma("sp", out[c, :, bs], x[:], [x], [DB("out", c)])
        S.finish()
    return nc


def _fm(v):
    v = np.asarray(v, np.float32)
    lead = v.shape[:-1]
    return v.reshape(*lead, NCH, 128).transpose(len(lead) + 1, *range(len(lead)), len(lead)).reshape(128, -1)


def pack_shared(inp, layers=tuple(range(DEPTH)), nomoe=False):
    f = lambda k: np.asarray(inp[k], np.float32)
    ls = list(layers)
    vec = np.zeros((128, VOFF["_n"]), np.float32)

    def put(name, arr):
        vec[:, VOFF[name]:VOFF[name] + arr.shape[1]] = arr

    put("ada_b", f("ada_b").reshape(DEPTH, 6 * NCH, 128).transpose(2, 0, 1).reshape(128, -1))
    put("norm_mix", _fm(f("norm_mix")))
    put("norm_ffn", _fm(f("norm_ffn")))
    put("norm_final", _fm(f("norm_final")))
    put("s5_d", _fm(f("s5_d")))
    put("lb_raw", _fm(f("hg_lb_raw")))
    put("hg_norm", f("hg_norm").T.copy())
    put("b_gu", f("moe_b_gate_up").reshape(DEPTH, NE, 8, 128).transpose(3, 0, 1, 2).reshape(128, -1))
    dup = lambda a: np.concatenate([a, a], axis=2).transpose(2, 0, 1).reshape(128, -1)
    put("s5_lre", dup(f("s5_lambda_re")))
    put("s5_lim", dup(f("s5_lambda_im")))
    put("s5_ldt", np.broadcast_to(f("s5_log_dt").reshape(1, -1), (128, 256)).copy())
    bre, bim = f("s5_b_re"), f("s5_b_im")
    cre, cim = f("s5_c_re"), f("s5_c_im")

    def stackB(a, b):
        return np.concatenate([a, b], axis=2).transpose(0, 2, 1, 3).reshape(2, 128, -1)

    def stackC(a, b):
        return np.concatenate([a, b], axis=3).transpose(0, 3, 1, 2).reshape(2, 128, -1)

    s5B = np.stack([stackB(bre, bim), stackB(bim, bre)], axis=1)
    s5C = np.stack([stackC(cre, cim), stackC(cim, cre)], axis=1)
    shared = {
        "cst": CONST_NP,
        "ada_w": f("ada_w") if len(ls) == DEPTH else np.ascontiguousarray(f("ada_w")[ls]),
        "s5B": np.ascontiguousarray(s5B), "s5C": np.ascontiguousarray(s5C),
        "s5_w_glu": f("s5_w_glu"), "hg_w_in": f("hg_w_in"), "hg_w_out": f("hg_w_out"),
        "router_w": f("router_w"), "router_b": f("router_b").reshape(1, -1),
    }
    if nomoe:
        shared.update({"moe_w_gate_up": np.zeros((1, 1, 1, 1), np.float32), "moe_w_down": np.zeros((1, 1, 1, 1), np.float32),
                       "moe_b_down": np.zeros((1, 1, 1), np.float32)})
    elif len(ls) == DEPTH:
        shared.update({"moe_w_gate_up": f("moe_w_gate_up"), "moe_w_down": f("moe_w_down"), "moe_b_down": f("moe_b_down")})
    else:
        shared.update({"moe_w_gate_up": np.ascontiguousarray(f("moe_w_gate_up")[ls]),
                       "moe_w_down": np.ascontiguousarray(f("moe_w_down")[ls]),
                       "moe_b_down": np.ascontiguousarray(f("moe_b_down")[ls])})
    return vec, shared


def core_inputs(inp, b, vec, shared, L):
    v = vec.copy()
    v[:, VOFF["cT"]:VOFF["cT"] + NCH] = np.asarray(inp["c"], np.float32)[b].reshape(NCH, 128).T
    x = np.asarray(inp["x"], np.float32)[b, :L]
    m = dict(shared)
    m["vec"] = v
    m["xT"] = np.ascontiguousarray(x.T.reshape(NCH, 128, L))
    return m


_NC_CACHE = {}


def kernel(**inputs):
    B, L, _ = inputs["x"].shape
    vec, shared = pack_shared(inputs)
    if L not in _NC_CACHE:
        _NC_CACHE[L] = build(L)
    nc = _NC_CACHE[L]
    in_maps = [core_inputs(inputs, b, vec, shared, L) for b in range(B)]
    res = run_bass_kernel_spmd(nc, in_maps, core_ids=list(range(B)))
    outs = [np.asarray(r["outT"]).reshape(D, L).T for r in res.results]
    return np.ascontiguousarray(np.stack(outs, axis=0).astype(np.float32))
```

```python
import contextlib
import math
import numpy as np
import concourse.bass as bass
import concourse.mybir as mybir
from concourse.bass_utils import run_bass_kernel_spmd

F32 = mybir.dt.float32
BF16 = mybir.dt.bfloat16
AF = mybir.ActivationFunctionType
ALU = mybir.AluOpType
AX = mybir.AxisListType

D = 2048
NCH = 16
NE = 32
FF = 512
DEPTH = 4
EPS = 1e-6
TB = 512
MAGIC = 12582912.0
TWO_PI = 2.0 * math.pi
SEM_LIMIT = 30000


ALL_BUFS = []


class Buf:
    __slots__ = ("w", "r", "frozen")

    def __init__(self):
        self.w = None
        self.r = []
        self.frozen = False
        ALL_BUFS.append(self)


class T:
    def __init__(self, t):
        self.t = t
        self.b = Buf()

    def __getitem__(self, k):
        return self.t[k]


class Sched:
    def __init__(self, nc, es):
        self.nc = nc
        self.es = es
        self.ops = []
        self.last_op = {}
        self.engs = {"pe": nc.tensor, "act": nc.scalar, "dve": nc.vector, "pool": nc.gpsimd, "sp": nc.sync}

    def op(self, eng, fn, reads=(), writes=(), dma=False):
        idx = len(self.ops)
        deps = set()
        for b in reads:
            b = b.b if isinstance(b, T) else b
            if b.w is not None:
                deps.add(b.w)
        for b in writes:
            b = b.b if isinstance(b, T) else b
            if b.w is not None:
                deps.add(b.w)
            deps.update(b.r)
        for b in reads:
            b = b.b if isinstance(b, T) else b
            if not b.frozen:
                b.r.append(idx)
        for b in writes:
            b = b.b if isinstance(b, T) else b
            b.w = idx
            b.r = []
        self.ops.append((eng, fn, deps, dma))
        self.last_op[eng] = idx
        return idx

    def _init_emit(self):
        nc, es = self.nc, self.es
        self.done = 0
        self.sem_of = {}
        self.cur = {}
        self.dma_pool = [es.enter_context(nc.semaphore(f"dq{i}")) for i in range(32)]
        self.dma_cnt = [0] * len(self.dma_pool)
        self.dma_rr = 0
        self.retired = []
        self.waited = {e: {} for e in self.engs}
        self.nsem = 0

    def flush(self):
        nc, es = self.nc, self.es
        if not hasattr(self, "done"):
            self._init_emit()
        ops = self.ops
        n = len(ops)
        lo = self.done
        needs = set()
        for i in range(lo, n):
            eng, fn, deps, dma = ops[i]
            for d in deps:
                deng, _, _, ddma = ops[d]
                if ddma or deng != eng or eng != "pe":
                    needs.add(d)
        for eng_, li_ in self.last_op.items():
            if li_ >= lo and not ops[li_][3]:
                needs.add(li_)
        for b in ALL_BUFS:
            if b.w is not None:
                needs.add(b.w)
            needs.update(b.r)
        sem_of = self.sem_of
        for i in range(lo, n):
            eng, fn, deps, dma = ops[i]
            h = self.engs[eng]
            w = self.waited[eng]
            pend = {}
            for d in deps:
                deng, _, _, ddma = ops[d]
                if (not ddma) and deng == eng and eng == "pe":
                    continue
                s, v = sem_of[d]
                if w.get(s, 0) < v and pend.get(s, 0) < v:
                    pend[s] = v
            if dma:
                k = self.dma_rr
                self.dma_rr = (self.dma_rr + 1) % len(self.dma_pool)
                if self.dma_cnt[k] + 16 > SEM_LIMIT:
                    self.retired.append((self.dma_pool[k], self.dma_cnt[k]))
                    self.dma_pool[k] = es.enter_context(nc.semaphore(f"dq{k}_{i}"))
                    self.dma_cnt[k] = 0
                dsem = self.dma_pool[k]
                if self.dma_cnt[k] > 0 and w.get(dsem, 0) < self.dma_cnt[k]:
                    pend[dsem] = max(pend.get(dsem, 0), self.dma_cnt[k])
            for ws, wv in pend.items():
                h.wait_ge(ws, wv)
                w[ws] = wv
            ins = fn()
            if dma:
                self.dma_cnt[k] += 16
                ins.then_inc(dsem, 16)
                sem_of[i] = (dsem, self.dma_cnt[k])
            elif i in needs:
                c = self.cur.get(eng)
                if c is None or c[1] + 1 > SEM_LIMIT:
                    c = [es.enter_context(nc.semaphore(f"e{eng}{self.nsem}")), 0]
                    self.nsem += 1
                    self.cur[eng] = c
                c[1] += 1
                ins.then_inc(c[0], 1)
                sem_of[i] = (c[0], c[1])
            ops[i] = (eng, None, deps, dma)
        self.done = n
        evs = []
        for eng_, li_ in self.last_op.items():
            if li_ in sem_of:
                evs.append(sem_of[li_])
        for k, s_ in enumerate(self.dma_pool):
            if self.dma_cnt[k] > 0:
                evs.append((s_, self.dma_cnt[k]))
        evs.extend(self.retired)
        for eng_, h in self.engs.items():
            w = self.waited[eng_]
            for s_, v_ in evs:
                if w.get(s_, 0) < v_:
                    h.wait_ge(s_, v_)
                    w[s_] = v_
        return n

    def finish(self):
        nc = self.nc
        self.flush()
        for k, s in enumerate(self.dma_pool):
            if self.dma_cnt[k] > 0:
                nc.sync.wait_ge(s, self.dma_cnt[k])
        for s, c in self.retired:
            nc.sync.wait_ge(s, c)
        for eng, c in self.cur.items():
            nc.sync.wait_ge(c[0], c[1])


def _layout():
    off = {}
    n = 0

    def add(name, w):
        nonlocal n
        off[name] = n
        n += w

    add("cT", NCH)
    add("ada_b", DEPTH * 96)
    add("norm_mix", DEPTH * NCH)
    add("norm_ffn", DEPTH * NCH)
    add("norm_final", NCH)
    add("s5_d", 2 * NCH)
    add("lb_raw", DEPTH * NCH)
    add("hg_norm", 2)
    add("b_gu", DEPTH * NE * 8)
    add("s5_lre", 2 * 128)
    add("s5_lim", 2 * 128)
    add("s5_ldt", 2 * 128)
    off["_n"] = n
    return off


VOFF = _layout()


def _consts():
    c = {}
    c["ident"] = np.eye(128, dtype=np.float32)
    c["ones"] = np.ones((128, 128), np.float32)
    J = np.zeros((128, 128), np.float32)
    for p in range(64):
        J[64 + p, p] = -1.0
        J[p, 64 + p] = 1.0
    c["J"] = J
    gm = np.zeros((128, 8), np.float32)
    for p in range(128):
        gm[p, p // 16] = 1.0
    c["gmask"] = gm
    sg = np.ones((128, 1), np.float32)
    sg[64:] = -1.0
    c["sgn"] = sg
    c["iota"] = np.tile(np.arange(TB + 1, dtype=np.float32)[None, :], (128, 1))
    c["cmask"] = np.tile((np.arange(TB) % 64 != 0).astype(np.float32)[None, :], (128, 1))
    s = np.arange(128)[:, None]
    t = np.arange(128)[None, :]
    c["bcmask"] = ((s // 64 == t // 64) & (s <= t)).astype(np.float32)
    names = ["ident", "ones", "J", "gmask", "sgn", "iota", "cmask", "bcmask"]
    offs = {}
    n = 0
    for k in names:
        offs[k] = n
        n += c[k].shape[1]
    return np.concatenate([c[k] for k in names], axis=1), offs


CONST_NP, COFF = _consts()


def build(L, layers=tuple(range(DEPTH)), dbg=()):
    NB = L // TB
    del ALL_BUFS[:]
    nc = bass.Bass("TRN2", target_bir_lowering=False)
    dram = {}
    dbufs = {}

    def din(name, shape, dt=F32):
        dram[name] = nc.dram_tensor(name, list(shape), dt, kind="ExternalInput").ap()
        return dram[name]

    def dscr(name, shape, dt=F32):
        kind = "ExternalOutput" if name in dbg else "Internal"
        dram[name] = nc.dram_tensor(name, list(shape), dt, kind=kind).ap()
        return dram[name]

    def DB(*key):
        if key not in dbufs:
            dbufs[key] = Buf()
        return dbufs[key]

    xin = din("xT", [NCH, 128, L])
    vec = din("vec", [128, VOFF["_n"]])
    cst = din("cst", [128, CONST_NP.shape[1]])
    NL = len(layers)
    LI = {l: n_ for n_, l in enumerate(layers)}
    nomoe = "nomoe" in dbg
    ada_w = din("ada_w", [NL, D, 6 * D])
    s5B = din("s5B", [2, 2, 128, 128 * 16])
    s5C = din("s5C", [2, 2, 128, 128 * 16])
    w_glu = din("s5_w_glu", [2, D, 2 * D])
    w_in = din("hg_w_in", [2, D, 4 * D])
    w_out = din("hg_w_out", [2, D, D])
    router_w = din("router_w", [DEPTH, D, NE])
    router_b = din("router_b", [1, DEPTH * NE])
    w_gu = din("moe_w_gate_up", [1, 1, 1, 1] if nomoe else [NL, NE, D, 2 * FF])
    w_dn = din("moe_w_down", [1, 1, 1, 1] if nomoe else [NL, NE, FF, D])
    b_dn = din("moe_b_down", [1, 1, 1] if nomoe else [NL, NE, D])
    out = nc.dram_tensor("outT", [NCH, 128, L], F32, kind="ExternalOutput").ap()

    xT = dscr("xTs", [NCH, 128, L])
    actT = dscr("actT", [NCH, 128, L], BF16)
    hT = dscr("hT", [NCH, 128, L], BF16)
    ymoe = dscr("ymoe", [NCH, 128, L])

    with contextlib.ExitStack() as es:
        S = Sched(nc, es)

        @contextlib.contextmanager
        def scope():
            with contextlib.ExitStack() as e:
                yield e
                S.flush()

        uid = [0]

        def sb(es_, name, shape, dt=F32):
            uid[0] += 1
            return T(es_.enter_context(nc.sbuf_tensor(f"{name}_{uid[0]}", list(shape), dt)))

        def ps(es_, name, shape=(128, 512), dt=F32):
            uid[0] += 1
            return T(es_.enter_context(nc.psum_tensor(f"{name}_{uid[0]}", [128, 512 if dt == F32 else 1024], dt)))

        def dma(q, out_ap, in_ap, reads, writes, **kw):
            h = {"sp": nc.sync, "pool": nc.gpsimd}[q]
            S.op(q, lambda: h.dma_start(out=out_ap, in_=in_ap, **kw), reads=reads, writes=writes, dma=True)

        def dump(name, t, shape, ap=None):
            if ("dump" not in dbg) or name in dram:
                return
            d = nc.dram_tensor("dbg_" + name, list(shape), F32, kind="ExternalOutput").ap()
            dram[name] = d
            dma("sp", d[:, :], t[:] if ap is None else ap, [t], [Buf()])

        V = sb(es, "V", [128, VOFF["_n"]])
        C = sb(es, "C", [128, CONST_NP.shape[1]])
        dma("sp", V[:], vec[:, :], [], [V])
        dma("sp", C[:], cst[:, :], [], [C])

        def vcol(name, i=0, w=1):
            o = VOFF[name] + i
            return V[:, o:o + w]

        def ccol(name, i=0, w=None):
            o = COFF[name] + i
            if w is None:
                w = {"ident": 128, "ones": 128, "J": 128, "gmask": 8, "sgn": 1, "iota": TB + 1, "cmask": TB,
                     "bcmask": 128}[name] - i
            return C[:, o:o + w]

        ident = ccol("ident")
        ones = ccol("ones")
        ZB = sb(es, "ZB", [128, 512], BF16)
        S.op("dve", lambda: nc.vector.memset(ZB[:], 0.0), writes=[ZB])

        def fence(out_ap, M, N, reads, writes):
            S.op("pe", lambda: nc.tensor.matmul(out_ap, lhsT=ZB[:, 0:M], rhs=ZB[:, 0:N], start=False, stop=True),
                 reads=list(reads) + [ZB], writes=writes)

        MOD = sb(es, "MOD", [128, DEPTH * 96])
        AB = sb(es, "AB", [128, DEPTH * 64])
        LB = sb(es, "LB", [128, DEPTH * NCH * 2])
        ones_bf = sb(es, "ones_bf", [128, 128], BF16)
        onesD = sb(es, "onesD", [128, 128])
        S.op("dve", lambda: nc.vector.memset(onesD[:], 1.0 / 128.0), writes=[onesD])

        with scope() as e1:
            xb = [sb(e1, f"xcp{i}", [128, L]) for i in range(2)]
            for c in range(NCH):
                t = xb[c % 2]
                dma("sp", t[:], xin[c, :, :], [], [t])
                dma("sp", xT[c, :, :], t[:], [t], [DB("x", c)])

        with scope() as e1:
            cact = sb(e1, "cact", [128, NCH])
            S.op("act", lambda: nc.scalar.activation(out=cact[:], in_=vcol("cT", 0, NCH), func=AF.Silu),
                 reads=[V], writes=[cact])
            wt = [sb(e1, f"adaw{i}", [128, NCH, 512]) for i in range(2)]
            mps = ps(e1, "modps", [128, 512])
            nld = 0
            for i in layers:
                for cb in range(24):
                    w = wt[nld % 2]
                    nld += 1
                    src = ada_w[LI[i], :, cb * 512:(cb + 1) * 512].rearrange("(k p) n -> p k n", p=128)
                    dma("sp", w[:], src, [], [w])
                    for q in range(4):
                        col = cb * 4 + q
                        for k in range(NCH):
                            S.op("pe", (lambda w=w, q=q, k=k, col=col: nc.tensor.matmul(
                                mps[:, col:col + 1], lhsT=w[:, k, q * 128:(q + 1) * 128], rhs=cact[:, k:k + 1],
                                start=(k == 0), stop=(k == NCH - 1))), reads=[w, cact], writes=[mps])
                fence(mps[:, 95:96], 128, 1, [], [mps])
                S.op("dve", (lambda i=i: nc.vector.tensor_tensor(
                    out=MOD[:, i * 96:(i + 1) * 96], in0=mps[:, 0:96], in1=vcol("ada_b", i * 96, 96), op=ALU.add)),
                     reads=[mps, V], writes=[MOD])
                dump("cact", cact, [128, NCH]); dump("wlast", w, [128, NCH * 512], w[:].rearrange("p k n -> p (k n)"))
                dump("MOD0", MOD, [128, DEPTH * 96])
                for j, nm in ((0, "norm_mix"), (1, "norm_ffn")):
                    S.op("dve", (lambda i=i, j=j, nm=nm: nc.vector.scalar_tensor_tensor(
                        out=AB[:, i * 64 + j * 16: i * 64 + j * 16 + 16],
                        in0=MOD[:, i * 96 + (1 + 3 * j) * 16: i * 96 + (1 + 3 * j) * 16 + 16], scalar=1.0,
                        in1=vcol(nm, i * NCH, NCH), op0=ALU.add, op1=ALU.mult)), reads=[MOD, V], writes=[AB])

        def modc(i, j, c):
            o = i * 96 + j * 16 + c
            return MOD[:, o:o + 1]

        def acol(i, j, c):
            o = i * 64 + j * 16 + c
            return AB[:, o:o + 1]

        with scope() as e1:
            ex = sb(e1, "lbex", [128, DEPTH * NCH])
            mx = sb(e1, "lbmx", [128, NCH])
            sm = sb(e1, "lbsm", [128, NCH])
            raw = lambda l: vcol("lb_raw", l * NCH, NCH)
            S.op("dve", lambda: nc.vector.tensor_max(out=mx[:], in0=raw(0), in1=raw(1)), reads=[V], writes=[mx])
            S.op("dve", lambda: nc.vector.tensor_max(out=mx[:], in0=mx[:], in1=raw(2)), reads=[V, mx], writes=[mx])
            S.op("dve", lambda: nc.vector.tensor_max(out=mx[:], in0=mx[:], in1=raw(3)), reads=[V, mx], writes=[mx])
            for l in range(DEPTH):
                S.op("dve", (lambda l=l: nc.vector.tensor_tensor(out=ex[:, l * NCH:(l + 1) * NCH], in0=raw(l), in1=mx[:],
                                                                 op=ALU.subtract)), reads=[V, mx], writes=[ex])
            S.op("act", lambda: nc.scalar.activation(out=ex[:], in_=ex[:], func=AF.Exp), reads=[ex], writes=[ex])
            S.op("dve", lambda: nc.vector.tensor_tensor(out=sm[:], in0=ex[:, 0:NCH], in1=ex[:, NCH:2 * NCH], op=ALU.add),
                 reads=[ex], writes=[sm])
            S.op("dve", lambda: nc.vector.tensor_tensor(out=sm[:], in0=sm[:], in1=ex[:, 2 * NCH:3 * NCH], op=ALU.add),
                 reads=[ex, sm], writes=[sm])
            S.op("dve", lambda: nc.vector.tensor_tensor(out=sm[:], in0=sm[:], in1=ex[:, 3 * NCH:4 * NCH], op=ALU.add),
                 reads=[ex, sm], writes=[sm])
            S.op("dve", lambda: nc.vector.reciprocal(out=sm[:], in_=sm[:]), reads=[sm], writes=[sm])
            for l in range(DEPTH):
                S.op("dve", (lambda l=l: nc.vector.tensor_tensor(out=ex[:, l * NCH:(l + 1) * NCH],
                                                                 in0=ex[:, l * NCH:(l + 1) * NCH], in1=sm[:], op=ALU.mult)),
                     reads=[ex, sm], writes=[ex])
            S.op("dve", lambda: nc.vector.memset(LB[:, 0:NCH], 0.0), writes=[LB])
            for l in range(1, DEPTH):
                S.op("dve", (lambda l=l: nc.vector.tensor_tensor(out=LB[:, l * NCH:(l + 1) * NCH],
                                                                 in0=LB[:, (l - 1) * NCH:l * NCH],
                                                                 in1=ex[:, l * NCH:(l + 1) * NCH], op=ALU.add)),
                     reads=[LB, ex], writes=[LB])
            S.op("dve", lambda: nc.vector.tensor_scalar(LB[:, DEPTH * NCH:2 * DEPTH * NCH], LB[:, 0:DEPTH * NCH], -1.0, 1.0,
                                                        op0=ALU.mult, op1=ALU.add), reads=[LB], writes=[LB])

        FREEZE = [V, C, MOD, AB, LB, onesD]
        def rstd_phase(e1, src):
            rstd = sb(e1, "rstd", [128, L])
            with scope() as e2:
                xt = [sb(e2, f"rs_x{i}", [128, TB]) for i in range(3)]
                sq = [sb(e2, f"rs_sq{i}", [128, TB]) for i in range(2)]
                pss = [ps(e2, f"rs_ps{i}") for i in range(2)]
                tmp = sb(e2, "rs_tmp", [128, TB])
                n = 0
                for blk in range(NB):
                    p = pss[blk % 2]
                    for c in range(NCH):
                        x = xt[n % 3]
                        q = sq[n % 2]
                        n += 1
                        dma("sp", x[:], src[c, :, blk * TB:(blk + 1) * TB], [DB("x", c)], [x])
                        S.op("act", (lambda x=x, q=q: nc.scalar.activation(out=q[:], in_=x[:], func=AF.Square)),
                             reads=[x], writes=[q])
                        S.op("pe", (lambda p=p, q=q, c=c: nc.tensor.matmul(p[:], lhsT=ones, rhs=q[:], start=(c == 0),
                                                                          stop=(c == NCH - 1))), reads=[q, C], writes=[p])
                    fence(p[:], 128, TB, [], [p])
                    S.op("act", (lambda p=p: nc.scalar.activation(out=tmp[:], in_=p[:], func=AF.Sqrt, scale=1.0 / D,
                                                                   bias=epsc[:, 0:1])), reads=[p, epsc], writes=[tmp])
                    S.op("dve", (lambda blk=blk: nc.vector.reciprocal(out=rstd[:, blk * TB:(blk + 1) * TB], in_=tmp[:])),
                         reads=[tmp], writes=[rstd])
            return rstd

        epsc = sb(es, "epsc", [128, 1])
        S.op("dve", lambda: nc.vector.memset(epsc[:], EPS), writes=[epsc])

        def s5_layer(i):
            j = i // 2
            with scope() as e1:
                rstd = rstd_phase(e1, xT)
                nm = ["lr", "li", "dt", "wr", "wi", "t0", "t1", "t2", "sn", "cs", "mg", "lbr", "lbi", "den", "cr", "ci",
                      "ncis", "ncrs", "nci"]
                cf = {k: sb(e1, "s5c_" + k, [128, 128]) for k in nm}
                lre = vcol("s5_lre", j * 128, 128)
                lim = vcol("s5_lim", j * 128, 128)
                ldt = vcol("s5_ldt", j * 128, 128)
                sgn = ccol("sgn")
                vop = lambda fn, r, w: S.op("dve", fn, reads=r, writes=w)
                vop(lambda: nc.vector.tensor_scalar(cf["lr"][:], lre, -1e-4, None, op0=ALU.min), [V], [cf["lr"]])
                S.op("act", lambda: nc.scalar.activation(out=cf["dt"][:], in_=ldt, func=AF.Exp), reads=[V], writes=[cf["dt"]])
                vop(lambda: nc.vector.tensor_tensor(out=cf["wr"][:], in0=cf["lr"][:], in1=cf["dt"][:], op=ALU.mult),
                    [cf["lr"], cf["dt"]], [cf["wr"]])
                vop(lambda: nc.vector.tensor_tensor(out=cf["wi"][:], in0=lim, in1=cf["dt"][:], op=ALU.mult),
                    [V, cf["dt"]], [cf["wi"]])

                def sincos(theta, sn_out, cs_out, shape_t, rd, tmp0, tmp1):
                    for shift, o in ((0.0, sn_out), (0.5 * math.pi, cs_out)):
                        vop(lambda shift=shift: nc.vector.tensor_scalar(tmp0, theta, shift, 1.0 / TWO_PI, op0=ALU.add,
                                                                         op1=ALU.mult), rd, shape_t)
                        vop(lambda: nc.vector.tensor_scalar(tmp1, tmp0, MAGIC, MAGIC, op0=ALU.add, op1=ALU.subtract),
                            shape_t, shape_t)
                        vop(lambda: nc.vector.tensor_tensor(out=tmp0, in0=tmp0, in1=tmp1, op=ALU.subtract), shape_t, shape_t)
                        S.op("act", (lambda o=o: nc.scalar.activation(out=o, in_=tmp0, func=AF.Sin, scale=TWO_PI * 0.999999)),
                             reads=shape_t, writes=shape_t)

                tl = [cf["t0"], cf["t1"], cf["sn"], cf["cs"]]
                sincos(cf["wi"][:], cf["sn"][:], cf["cs"][:], tl, [cf["wi"]] + tl, cf["t0"][:], cf["t1"][:])
                S.op("act", lambda: nc.scalar.activation(out=cf["mg"][:], in_=cf["wr"][:], func=AF.Exp),
                     reads=[cf["wr"]], writes=[cf["mg"]])
                vop(lambda: nc.vector.tensor_tensor(out=cf["lbr"][:], in0=cf["cs"][:], in1=cf["mg"][:], op=ALU.mult),
                    [cf["cs"], cf["mg"]], [cf["lbr"]])
                vop(lambda: nc.vector.tensor_tensor(out=cf["lbi"][:], in0=cf["sn"][:], in1=cf["mg"][:], op=ALU.mult),
                    [cf["sn"], cf["mg"]], [cf["lbi"]])
                vop(lambda: nc.vector.tensor_tensor(out=cf["den"][:], in0=cf["lr"][:], in1=cf["lr"][:], op=ALU.mult),
                    [cf["lr"]], [cf["den"]])
                vop(lambda: nc.vector.tensor_tensor(out=cf["t0"][:], in0=lim, in1=lim, op=ALU.mult), [V], [cf["t0"]])
                vop(lambda: nc.vector.tensor_tensor(out=cf["den"][:], in0=cf["den"][:], in1=cf["t0"][:], op=ALU.add),
                    [cf["den"], cf["t0"]], [cf["den"]])
                vop(lambda: nc.vector.reciprocal(out=cf["den"][:], in_=cf["den"][:]), [cf["den"]], [cf["den"]])
                vop(lambda: nc.vector.tensor_scalar(cf["t2"][:], cf["lbr"][:], -1.0, None, op0=ALU.add), [cf["lbr"]], [cf["t2"]])
                vop(lambda: nc.vector.tensor_tensor(out=cf["t0"][:], in0=cf["t2"][:], in1=cf["lr"][:], op=ALU.mult),
                    [cf["t2"], cf["lr"]], [cf["t0"]])
                vop(lambda: nc.vector.tensor_tensor(out=cf["t1"][:], in0=cf["lbi"][:], in1=lim, op=ALU.mult),
                    [cf["lbi"], V], [cf["t1"]])
                vop(lambda: nc.vector.tensor_tensor(out=cf["t0"][:], in0=cf["t0"][:], in1=cf["t1"][:], op=ALU.add),
                    [cf["t0"], cf["t1"]], [cf["t0"]])
                vop(lambda: nc.vector.tensor_tensor(out=cf["cr"][:], in0=cf["t0"][:], in1=cf["den"][:], op=ALU.mult),
                    [cf["t0"], cf["den"]], [cf["cr"]])
                vop(lambda: nc.vector.tensor_tensor(out=cf["t0"][:], in0=cf["lbi"][:], in1=cf["lr"][:], op=ALU.mult),
                    [cf["lbi"], cf["lr"]], [cf["t0"]])
                vop(lambda: nc.vector.tensor_tensor(out=cf["t1"][:], in0=cf["t2"][:], in1=lim, op=ALU.mult),
                    [cf["t2"], V], [cf["t1"]])
                vop(lambda: nc.vector.tensor_tensor(out=cf["t0"][:], in0=cf["t0"][:], in1=cf["t1"][:], op=ALU.subtract),
                    [cf["t0"], cf["t1"]], [cf["t0"]])
                vop(lambda: nc.vector.tensor_tensor(out=cf["ci"][:], in0=cf["t0"][:], in1=cf["den"][:], op=ALU.mult),
                    [cf["t0"], cf["den"]], [cf["ci"]])
                vop(lambda: nc.vector.tensor_scalar(cf["ncis"][:], cf["ci"][:], sgn, -1.0, op0=ALU.mult, op1=ALU.mult),
                    [cf["ci"], C], [cf["ncis"]])
                vop(lambda: nc.vector.tensor_scalar(cf["ncrs"][:], cf["cr"][:], sgn, None, op0=ALU.mult),
                    [cf["cr"], C], [cf["ncrs"]])
                vop(lambda: nc.vector.tensor_copy(out=cf["nci"][:], in_=cf["ci"][:]), [cf["ci"]], [cf["nci"]])
                dump("MOD", MOD, [128, DEPTH * 96]); dump("rstd", rstd, [128, L])
                for k_ in ("wr", "wi", "sn", "cs", "lbr", "lbi", "cr", "ci"):
                    dump("cf_" + k_, cf[k_], [128, 128])
                nwr = cf["den"]
                vop(lambda: nc.vector.tensor_scalar(nwr[:], cf["wr"][:], -1.0, None, op0=ALU.mult), [cf["wr"]], [nwr])

                with scope() as e2:
                    xc = [sb(e2, f"s5x{k}", [128, L]) for k in range(1)]
                    u32 = sb(e2, "s5u32", [128, L])
                    ub = sb(e2, "s5ub", [128, L], BF16)
                    yacc = sb(e2, "s5yacc", [128, L])
                    zb = [sb(e2, f"s5zb{k}", [128, L], BF16) for k in range(1)]
                    Bri = sb(e2, "s5Bri", [128, 128])
                    Bir = sb(e2, "s5Bir", [128, 128])
                    Cst = sb(e2, "s5Cst", [128, 128])
                    Csw = sb(e2, "s5Csw", [128, 128])
                    B1s = sb(e2, "s5B1s", [128, 128])
                    B2s = sb(e2, "s5B2s", [128, 128])
                    bt = sb(e2, "s5bt", [128, 16])
                    B1g = sb(e2, "s5B1g", [128, 8, 128], BF16)
                    B2g = sb(e2, "s5B2g", [128, 8, 128], BF16)
                    CaZ = sb(e2, "s5CaZ", [128, 8, 128], BF16)
                    CbZ = sb(e2, "s5CbZ", [128, 8, 128], BF16)
                    S.op("pool", lambda: nc.gpsimd.memset(CaZ[:], 0.0), writes=[CaZ])
                    S.op("pool", lambda: nc.gpsimd.memset(CbZ[:], 0.0), writes=[CbZ])
                    tabs = [{k: sb(e2, f"s5t{q}_{k}", [128, TB + 1]) for k in ("Ere", "Eim", "Fre", "Fim")} for q in range(2)]
                    th = sb(e2, "s5th", [128, TB + 1])
                    tq0 = sb(e2, "s5tq0", [128, TB + 1])
                    tq1 = sb(e2, "s5tq1", [128, TB + 1])
                    snt = sb(e2, "s5snt", [128, TB + 1])
                    cst_ = sb(e2, "s5cst", [128, TB + 1])
                    mgE = sb(e2, "s5mgE", [128, TB + 1])
                    mgF = sb(e2, "s5mgF", [128, TB + 1])
                    W1 = [sb(e2, f"s5W1{k}", [128, TB]) for k in range(2)]
                    W2 = [sb(e2, f"s5W2{k}", [128, TB]) for k in range(2)]
                    Pp = [sb(e2, f"s5P{k}", [128, TB]) for k in range(2)]
                    Q1 = [sb(e2, f"s5Q1{k}", [128, TB], BF16) for k in range(2)]
                    Q2 = [sb(e2, f"s5Q2{k}", [128, TB], BF16) for k in range(2)]
                    onesT = sb(e2, "s5ones", [128, TB])
                    S.op("pool", lambda: nc.gpsimd.memset(onesT[:], 1.0), writes=[onesT])
                    init = [sb(e2, f"s5init{k}", [128, 1]) for k in range(2)]
                    tmpc = sb(e2, "s5tmpc", [128, 1])
                    gt = sb(e2, "s5gt", [128, L])
                    pv1 = [ps(e2, f"s5pv1{k}") for k in range(2)]
                    pv2 = [ps(e2, f"s5pv2{k}") for k in range(2)]
                    pY = [ps(e2, f"s5pY{k}") for k in range(2)]
                    pT = ps(e2, "s5pT")
                    pJ = ps(e2, "s5pJ", [128, 8])
                    iota = ccol("iota")
                    nu = 0
                    pend = []
                    for c in range(NCH):
                        x = xc[0]
                        dma("sp", x[:], xT[c, :, :], [DB("x", c)], [x])
                        S.op("dve", (lambda x=x: nc.vector.tensor_tensor(out=u32[:], in0=x[:], in1=rstd[:], op=ALU.mult)),
                             reads=[x, rstd], writes=[u32])
                        S.op("act", (lambda c=c: nc.scalar.activation(out=u32[:], in_=u32[:], func=AF.Identity,
                                                                       scale=acol(i, 0, c), bias=modc(i, 0, c))),
                             reads=[u32, AB, MOD], writes=[u32])
                        S.op("act", lambda: nc.scalar.copy(out=ub[:], in_=u32[:]), reads=[u32], writes=[ub])
                        dump("u32", u32, [128, L])
                        gs = slice(c * 128, (c + 1) * 128)
                        dma("sp", Bri[:], s5B[j, 0, :, c * 128:(c + 1) * 128], [], [Bri])
                        dma("sp", Bir[:], s5B[j, 1, :, c * 128:(c + 1) * 128], [], [Bir])
                        dma("sp", Cst[:], s5C[j, 0, :, c * 128:(c + 1) * 128], [], [Cst])
                        dma("sp", Csw[:], s5C[j, 1, :, c * 128:(c + 1) * 128], [], [Csw])
                        for g in range(8):
                            G = c * 8 + g
                            hs = slice(g * 16, (g + 1) * 16)
                            vop(lambda G=G, hs=hs: nc.vector.tensor_scalar(bt[:], Bir[:, hs], cf["ncis"][:, G:G + 1], None,
                                                                             op0=ALU.mult), [Bir, cf["ncis"]], [bt])
                            vop(lambda G=G, hs=hs: nc.vector.scalar_tensor_tensor(out=B1s[:, hs], in0=Bri[:, hs],
                                                                                    scalar=cf["cr"][:, G:G + 1], in1=bt[:],
                                                                                    op0=ALU.mult, op1=ALU.add),
                                [Bri, cf["cr"], bt], [B1s])
                            vop(lambda G=G, hs=hs: nc.vector.tensor_scalar(bt[:], Bri[:, hs], cf["nci"][:, G:G + 1], None,
                                                                             op0=ALU.mult), [Bri, cf["nci"]], [bt])
                            vop(lambda G=G, hs=hs: nc.vector.scalar_tensor_tensor(out=B2s[:, hs], in0=Bir[:, hs],
                                                                                    scalar=cf["ncrs"][:, G:G + 1], in1=bt[:],
                                                                                    op0=ALU.mult, op1=ALU.add),
                                [Bir, cf["ncrs"], bt], [B2s])
                            vop(lambda g=g, hs=hs: nc.vector.tensor_scalar(CaZ[:, g, hs], Cst[:, hs], sgn, None, op0=ALU.mult),
                                [Cst, C], [CaZ])
                            vop(lambda g=g, hs=hs: nc.vector.tensor_scalar(CbZ[:, g, hs], Csw[:, hs], -1.0, None, op0=ALU.mult),
                                [Csw], [CbZ])
                        for Bs, Bg in ((B1s, B1g), (B2s, B2g)):
                            S.op("pe", (lambda Bs=Bs: nc.tensor.transpose(pT[:, 0:128], Bs[:], ident)), reads=[Bs, C], writes=[pT])
                            fence(pT[:, 0:128], 128, 128, [], [pT])
                            for g in range(8):
                                vop(lambda Bg=Bg, g=g: nc.vector.tensor_scalar(Bg[:, g, :], pT[:, 0:128], ccol("gmask", g, 1),
                                                                                 None, op0=ALU.mult), [pT, C], [Bg])
                        for g in range(8):
                            G = c * 8 + g
                            tb = tabs[G % 2]
                            wtl = [th, tq0, tq1, snt, cst_]
                            vop(lambda G=G: nc.vector.tensor_scalar(th[:], iota, cf["wi"][:, G:G + 1], None, op0=ALU.mult),
                                [C, cf["wi"]], [th])
                            sincos(th[:], snt[:], cst_[:], wtl, wtl, tq0[:], tq1[:])
                            S.op("act", (lambda G=G: nc.scalar.activation(out=mgF[:], in_=iota, func=AF.Exp,
                                                                           scale=cf["wr"][:, G:G + 1])),
                                 reads=[C, cf["wr"]], writes=[mgF])
                            S.op("act", (lambda G=G: nc.scalar.activation(out=mgE[:], in_=iota, func=AF.Exp,
                                                                           scale=nwr[:, G:G + 1])),
                                 reads=[C, nwr], writes=[mgE])
                            S.op("dve", (lambda tb=tb: nc.vector.tensor_tensor(out=tb["Ere"][:], in0=cst_[:], in1=mgE[:],
                                                                                 op=ALU.mult)), reads=[cst_, mgE], writes=[tb["Ere"]])
                            S.op("dve", (lambda tb=tb: nc.vector.tensor_tensor(out=tb["Eim"][:], in0=snt[:], in1=mgE[:],
                                                                                 op=ALU.mult)), reads=[snt, mgE], writes=[tb["Eim"]])
                            S.op("dve", (lambda tb=tb: nc.vector.tensor_tensor(out=tb["Fre"][:], in0=cst_[:], in1=mgF[:],
                                                                                 op=ALU.mult)), reads=[cst_, mgF], writes=[tb["Fre"]])
                            S.op("dve", (lambda tb=tb: nc.vector.tensor_tensor(out=tb["Fim"][:], in0=snt[:], in1=mgF[:],
                                                                                 op=ALU.mult)), reads=[snt, mgF], writes=[tb["Fim"]])
                            ini = init[0]
                            S.op("dve", (lambda ini=ini: nc.vector.memset(ini[:], 0.0)), writes=[ini])
                            for k_ in ("Ere", "Eim", "Fre", "Fim"):
                                dump("tab_" + k_, tb[k_], [128, TB + 1])
                            dump("B1s", B1s, [128, 128]); dump("B2s", B2s, [128, 128])
                            for blk in range(NB):
                                k = nu % 2
                                nu += 1
                                bs = slice(blk * TB, (blk + 1) * TB)
                                S.op("pe", (lambda k=k, g=g, bs=bs: nc.tensor.matmul(pv1[k][:], lhsT=B1g[:, g, :], rhs=ub[:, bs],
                                                                                      start=True, stop=True)),
                                     reads=[B1g, ub], writes=[pv1[k]])
                                S.op("pe", (lambda k=k, g=g, bs=bs: nc.tensor.matmul(pv2[k][:], lhsT=B2g[:, g, :], rhs=ub[:, bs],
                                                                                      start=True, stop=True)),
                                     reads=[B2g, ub], writes=[pv2[k]])
                                vop(lambda k=k, tb=tb: nc.vector.tensor_tensor(out=W1[k][:], in0=pv1[k][:], in1=tb["Ere"][:, 0:TB],
                                                                               op=ALU.mult), [pv1[k], tb["Ere"]], [W1[k]])
                                vop(lambda k=k, tb=tb: nc.vector.tensor_tensor(out=W2[k][:], in0=pv2[k][:], in1=tb["Eim"][:, 0:TB],
                                                                               op=ALU.mult), [pv2[k], tb["Eim"]], [W2[k]])
                                for f_ in pend:
                                    f_()
                                del pend[:]
                                cur_ini = init[blk % 2]
                                vop(lambda k=k, cur_ini=cur_ini: nc.vector.tensor_tensor_scan(
                                    out=Pp[k][:], data0=W2[k][:], data1=W1[k][:], initial=cur_ini[:, 0:1], op0=ALU.add,
                                    op1=ALU.add), [W2[k], W1[k], cur_ini], [Pp[k]])
                                if blk + 1 < NB:
                                    nxt = init[(blk + 1) % 2]
                                    S.op("pe", (lambda k=k: nc.tensor.matmul(pJ[:, 0:1], lhsT=ccol("J"), rhs=Pp[k][:, TB - 1:TB],
                                                                             start=True, stop=True)),
                                         reads=[Pp[k], C], writes=[pJ])
                                    fence(pJ[:, 0:1], 128, 1, [], [pJ])
                                S.op("dve", (lambda k=k, tb=tb: nc.vector.tensor_tensor(out=Q1[k][:], in0=Pp[k][:],
                                                                                          in1=tb["Fre"][:, 0:TB], op=ALU.mult)),
                                     reads=[Pp[k], tb["Fre"]], writes=[Q1[k]])
                                vop(lambda k=k, tb=tb: nc.vector.tensor_tensor(out=Q2[k][:], in0=Pp[k][:], in1=tb["Fim"][:, 0:TB],
                                                                               op=ALU.mult), [Pp[k], tb["Fim"]], [Q2[k]])
                                S.op("pe", (lambda k=k, g=g: nc.tensor.matmul(pY[k][:], lhsT=CaZ[:, g, :], rhs=Q1[k][:],
                                                                              start=True, stop=False)),
                                     reads=[CaZ, Q1[k]], writes=[pY[k]])
                                S.op("pe", (lambda k=k, g=g: nc.tensor.matmul(pY[k][:], lhsT=CbZ[:, g, :], rhs=Q2[k][:],
                                                                              start=False, stop=True)),
                                     reads=[CbZ, Q2[k]], writes=[pY[k]])
                                if blk + 1 < NB:
                                    vop(lambda tb=tb: nc.vector.tensor_tensor(out=tmpc[:], in0=pJ[:, 0:1], in1=tb["Fim"][:, TB:TB + 1],
                                                                              op=ALU.mult), [pJ, tb["Fim"]], [tmpc])
                                    vop(lambda k=k, tb=tb, nxt=nxt: nc.vector.scalar_tensor_tensor(
                                        out=nxt[:], in0=Pp[k][:, TB - 1:TB], scalar=tb["Fre"][:, TB:TB + 1], in1=tmpc[:],
                                        op0=ALU.mult, op1=ALU.add), [Pp[k], tb["Fre"], tmpc], [nxt])

                                def yacc_op(k=k, bs=bs, c=c, g=g):
                                    if g == 0:
                                        vop(lambda: nc.vector.scalar_tensor_tensor(
                                            out=yacc[:, bs], in0=u32[:, bs], scalar=vcol("s5_d", j * NCH + c), in1=pY[k][:],
                                            op0=ALU.mult, op1=ALU.add), [u32, V, pY[k]], [yacc])
                                    else:
                                        vop(lambda: nc.vector.tensor_tensor(out=yacc[:, bs], in0=yacc[:, bs], in1=pY[k][:],
                                                                            op=ALU.add), [yacc, pY[k]], [yacc])
                                pend.append(yacc_op)
                        for f_ in pend:
                            f_()
                        del pend[:]
                        dump("yacc", yacc, [128, L])
                        z = zb[0]
                        S.op("dve", lambda: nc.vector.tensor_tensor(out=gt[:], in0=yacc[:], in1=yacc[:], op=ALU.mult),
                             reads=[yacc], writes=[gt])
                        S.op("dve", lambda: nc.vector.tensor_scalar(gt[:], gt[:], 0.044715, 1.0, op0=ALU.mult, op1=ALU.add),
                             reads=[gt], writes=[gt])
                        S.op("dve", lambda: nc.vector.tensor_tensor(out=gt[:], in0=gt[:], in1=yacc[:], op=ALU.mult),
                             reads=[gt, yacc], writes=[gt])
                        S.op("act", lambda: nc.scalar.activation(out=gt[:], in_=gt[:], func=AF.Sigmoid, scale=1.5957691216),
                             reads=[gt], writes=[gt])
                        S.op("dve", (lambda z=z: nc.vector.tensor_tensor(out=z[:], in0=gt[:], in1=yacc[:], op=ALU.mult)),
                             reads=[gt, yacc], writes=[z])
                        dma("sp", actT[c, :, :], z[:], [z], [DB("act", c)])
            proj_phase(i, w_glu[j], 2 * D, glu=True)

        def proj_phase(i, wsrc, ncols, glu):
            with scope() as e1:
                A = sb(e1, "pjA", [128, NCH, L], BF16)
                for c in range(NCH):
                    dma("sp", A[:, c, :], actT[c, :, :], [DB("act", c)], [A])
                nW = 2 if glu else 1
                Wt = [[sb(e1, f"pjW{k}_{q}", [128, NCH, 128], BF16) for q in range(nW)] for k in range(2)]
                xt = [sb(e1, f"pjx{k}", [128, TB]) for k in range(3)]
                sg = [sb(e1, f"pjs{k}", [128, TB]) for k in range(2)]
                pa = [ps(e1, f"pjpa{k}") for k in range(2)]
                pg = [ps(e1, f"pjpg{k}") for k in range(2)]
                n = 0
                def load_w(jc):
                    for q in range(nW):
                        src = wsrc[:, q * D + jc * 128: q * D + (jc + 1) * 128].rearrange("(k p) n -> p k n", p=128)
                        dma("pool", Wt[jc % 2][q][:], src, [], [Wt[jc % 2][q]])

                load_w(0)
                for jc in range(NCH):
                    W = Wt[jc % 2]
                    if jc + 1 < NCH:
                        load_w(jc + 1)
                    for blk in range(NB):
                        bs = slice(blk * TB, (blk + 1) * TB)
                        k2 = n % 2
                        x = xt[n % 3]
                        n += 1
                        dma("sp", x[:], xT[jc, :, bs], [DB("x", jc)], [x])
                        for k in range(NCH):
                            S.op("pe", (lambda W=W, k=k, bs=bs, k2=k2: nc.tensor.matmul(
                                pa[k2][:], lhsT=W[0][:, k, :], rhs=A[:, k, bs], start=(k == 0), stop=(k == NCH - 1))),
                                 reads=[W[0], A], writes=[pa[k2]])
                        if glu:
                            for k in range(NCH):
                                S.op("pe", (lambda W=W, k=k, bs=bs, k2=k2: nc.tensor.matmul(
                                    pg[k2][:], lhsT=W[1][:, k, :], rhs=A[:, k, bs], start=(k == 0), stop=(k == NCH - 1))),
                                     reads=[W[1], A], writes=[pg[k2]])
                            S.op("act", (lambda k2=k2: nc.scalar.activation(out=sg[k2][:], in_=pg[k2][:], func=AF.Sigmoid)),
                                 reads=[pg[k2]], writes=[sg[k2]])
                            S.op("dve", (lambda k2=k2: nc.vector.tensor_tensor(out=sg[k2][:], in0=pa[k2][:], in1=sg[k2][:],
                                                                               op=ALU.mult)), reads=[pa[k2], sg[k2]], writes=[sg[k2]])
                            S.op("dve", (lambda k2=k2, x=x, jc=jc: nc.vector.scalar_tensor_tensor(
                                out=x[:], in0=sg[k2][:], scalar=modc(i, 2, jc), in1=x[:], op0=ALU.mult, op1=ALU.add)),
                                 reads=[sg[k2], MOD, x], writes=[x])
                        else:
                            S.op("dve", (lambda k2=k2, x=x, jc=jc: nc.vector.scalar_tensor_tensor(
                                out=x[:], in0=pa[k2][:], scalar=modc(i, 2, jc), in1=x[:], op0=ALU.mult, op1=ALU.add)),
                                 reads=[pa[k2], MOD, x], writes=[x])
                        dma("sp", xT[jc, :, bs], x[:], [x], [DB("x", jc)])

        def hg_layer(i):
            j = i // 2
            with scope() as e1:
                rstd = rstd_phase(e1, xT)
                with scope() as e2:
                    xc = [sb(e2, f"hgx{k}", [128, L]) for k in range(2)]
                    hb_ = [sb(e2, f"hgh{k}", [128, L], BF16) for k in range(2)]
                    for c in range(NCH):
                        x = xc[c % 2]
                        hb = hb_[c % 2]
                        dma("sp", x[:], xT[c, :, :], [DB("x", c)], [x])
                        S.op("dve", (lambda x=x: nc.vector.tensor_tensor(out=x[:], in0=x[:], in1=rstd[:], op=ALU.mult)),
                             reads=[x, rstd], writes=[x])
                        S.op("act", (lambda x=x, hb=hb, c=c: nc.scalar.activation(out=hb[:], in_=x[:], func=AF.Identity,
                                                                                   scale=acol(i, 0, c), bias=modc(i, 0, c))),
                             reads=[x, AB, MOD], writes=[hb])
                        dma("sp", hT[c, :, :], hb[:], [hb], [DB("h", c)])
            with scope() as e1:
                Wt = [[sb(e1, f"hgW{k}_{q}", [128, NCH, 128], BF16) for q in range(4)] for k in range(2)]
                hbk = [sb(e1, f"hghb{k}", [128, NCH, TB], BF16) for k in range(2)]
                S32 = [sb(e1, f"hgS32{k}", [128, 128]) for k in range(2)]
                Sbf = [sb(e1, f"hgSbf{k}", [128, 128], BF16) for k in range(2)]
                f32t = {k: sb(e1, "hg_" + k, [128, TB]) for k in ("sig", "f", "lf", "k", "b", "eb", "enb", "kk", "gs", "o", "sq", "r")}
                qt = sb(e1, "hg_qt", [128, TB], BF16)
                kt = sb(e1, "hg_kt", [128, TB], BF16)
                kh = sb(e1, "hg_kh", [128, TB], BF16)
                vtm = sb(e1, "hg_vtm", [128, 4, 128], BF16)
                khT = [sb(e1, f"hg_khT{k}", [128, 128], BF16) for k in range(2)]
                smk = [sb(e1, f"hg_sm{k}", [128, 128], BF16) for k in range(2)]
                onb = [sb(e1, f"hg_onb{k}", [128, TB], BF16) for k in range(2)]
                pq = ps(e1, "hgpq")
                pf = ps(e1, "hgpf")
                pgt = ps(e1, "hgpg")
                pv = ps(e1, "hgpv")
                pss = [ps(e1, f"hgps{k}", [128, 128]) for k in range(1)]
                pso = ps(e1, "hgpo", [128, 128])
                pst = ps(e1, "hgpt", [128, 128], BF16)
                pS = ps(e1, "hgpS", [128, 128])
                cmask = ccol("cmask")
                bcmask = ccol("bcmask")
                nb = 0
                def load_head(hd):
                    for q in range(4):
                        src = w_in[j][:, q * D + hd * 128: q * D + (hd + 1) * 128].rearrange("(k p) n -> p k n", p=128)
                        dma("pool", Wt[hd % 2][q][:], src, [], [Wt[hd % 2][q]])

                load_head(0)
                for hd in range(NCH):
                    W = Wt[hd % 2]
                    if hd + 1 < NCH:
                        load_head(hd + 1)
                    S.op("dve", lambda: nc.vector.memset(S32[0][:], 0.0), writes=[S32[0]])
                    S.op("dve", lambda: nc.vector.memset(Sbf[0][:], 0.0), writes=[Sbf[0]])
                    sidx = 0
                    lbc = LB[:, i * NCH + hd: i * NCH + hd + 1]
                    omlc = LB[:, DEPTH * NCH + i * NCH + hd: DEPTH * NCH + i * NCH + hd + 1]
                    for blk in range(NB):
                        bs = slice(blk * TB, (blk + 1) * TB)
                        hb = hbk[nb % 2]
                        ob = onb[nb % 2]
                        nb += 1
                        dma("sp", hb[:], hT[:, :, bs].rearrange("k p t -> p k t"), [DB("h", c) for c in range(NCH)], [hb])
                        for q, pp in ((0, pq), (1, pf), (3, pgt)):
                            for k in range(NCH):
                                S.op("pe", (lambda W=W, q=q, pp=pp, k=k, hb=hb: nc.tensor.matmul(
                                    pp[:], lhsT=W[q][:, k, :], rhs=hb[:, k, :], start=(k == 0), stop=(k == NCH - 1))),
                                     reads=[W[q], hb], writes=[pp])
                        for ts in range(4):
                            for k in range(NCH):
                                S.op("pe", (lambda W=W, ts=ts, k=k, hb=hb: nc.tensor.matmul(
                                    pv[:, ts * 128:(ts + 1) * 128], lhsT=hb[:, k, ts * 128:(ts + 1) * 128], rhs=W[2][:, k, :],
                                    start=(k == 0), stop=(k == NCH - 1))), reads=[W[2], hb], writes=[pv])
                        F = f32t
                        S.op("act", lambda: nc.scalar.copy(out=vtm[:].rearrange("p a b -> p (a b)"), in_=pv[:]), reads=[pv], writes=[vtm])
                        S.op("act", lambda: nc.scalar.activation(out=F["sig"][:], in_=pf[:], func=AF.Sigmoid), reads=[pf], writes=[F["sig"]])
                        S.op("dve", (lambda omlc=omlc, lbc=lbc: nc.vector.tensor_scalar(F["f"][:], F["sig"][:], omlc, lbc, op0=ALU.mult,
                                                                                         op1=ALU.add)), reads=[F["sig"], LB], writes=[F["f"]])
                        S.op("act", lambda: nc.scalar.activation(out=F["lf"][:], in_=F["f"][:], func=AF.Ln), reads=[F["f"]], writes=[F["lf"]])
                        S.op("dve", lambda: nc.vector.tensor_scalar(F["k"][:], F["f"][:], -1.0, 1.0, op0=ALU.mult, op1=ALU.add),
                             reads=[F["f"]], writes=[F["k"]])
                        S.op("dve", lambda: nc.vector.tensor_tensor_scan(out=F["b"][:], data0=cmask, data1=F["lf"][:], initial=0.0,
                                                                         op0=ALU.mult, op1=ALU.add), reads=[C, F["lf"]], writes=[F["b"]])
                        S.op("act", lambda: nc.scalar.activation(out=F["eb"][:], in_=F["b"][:], func=AF.Exp), reads=[F["b"]], writes=[F["eb"]])
                        S.op("act", lambda: nc.scalar.activation(out=F["enb"][:], in_=F["b"][:], func=AF.Exp, scale=-1.0),
                             reads=[F["b"]], writes=[F["enb"]])
                        S.op("dve", lambda: nc.vector.tensor_tensor(out=qt[:], in0=pq[:], in1=F["eb"][:], op=ALU.mult),
                             reads=[pq, F["eb"]], writes=[qt])
                        S.op("dve", lambda: nc.vector.tensor_tensor(out=F["kk"][:], in0=F["k"][:], in1=F["enb"][:], op=ALU.mult),
                             reads=[F["k"], F["enb"]], writes=[F["kk"]])
                        S.op("act", lambda: nc.scalar.copy(out=kt[:], in_=F["kk"][:]), reads=[F["kk"]], writes=[kt])
                        for ch in range(8):
                            cs_ = slice(ch * 64, (ch + 1) * 64)
                            S.op("dve", (lambda cs_=cs_, ch=ch: nc.vector.tensor_scalar(kh[:, cs_], F["kk"][:, cs_],
                                                                                         F["eb"][:, ch * 64 + 63: ch * 64 + 64], None,
                                                                                         op0=ALU.mult)), reads=[F["kk"], F["eb"]], writes=[kh])
                        S.op("act", lambda: nc.scalar.activation(out=F["gs"][:], in_=pgt[:], func=AF.Silu), reads=[pgt], writes=[F["gs"]])
                        for ts in range(4):
                            t0 = ts * 128
                            kT_ = khT[ts % 2]
                            sm_ = smk[ts % 2]
                            S.op("pe", (lambda t0=t0: nc.tensor.transpose(pst[:, 0:128], kh[:, t0:t0 + 128], ident_bf[:])), reads=[kh, ident_bf],
                                 writes=[pst])
                            S.op("act", (lambda kT_=kT_: nc.scalar.copy(out=kT_[:], in_=pst[:, 0:128])), reads=[pst], writes=[kT_])
                            S.op("pe", (lambda t0=t0: nc.tensor.matmul(pss[0][:, 0:128], lhsT=kt[:, t0:t0 + 128], rhs=qt[:, t0:t0 + 128],
                                                                      start=True, stop=True)), reads=[kt, qt], writes=[pss[0]])
                            S.op("dve", (lambda sm_=sm_: nc.vector.tensor_tensor(out=sm_[:], in0=pss[0][:, 0:128], in1=bcmask, op=ALU.mult)),
                                 reads=[pss[0], C], writes=[sm_])
                            S.op("pe", (lambda ts=ts, sm_=sm_: nc.tensor.matmul(pso[:, 0:128], lhsT=vtm[:, ts, :], rhs=sm_[:], start=True,
                                                                               stop=False)), reads=[vtm, sm_], writes=[pso])
                            for half in range(2):
                                hs = slice(half * 64, (half + 1) * 64)
                                sa = sidx % 2
                                sn_ = (sidx + 1) % 2
                                sidx += 1
                                S.op("pe", (lambda sa=sa, t0=t0, hs=hs, half=half: nc.tensor.matmul(
                                    pso[:, hs], lhsT=Sbf[sa][:], rhs=qt[:, t0 + half * 64: t0 + half * 64 + 64], start=False,
                                    stop=(half == 1))), reads=[Sbf[sa], qt], writes=[pso])
                                S.op("pe", (lambda kT_=kT_, hs=hs, ts=ts: nc.tensor.matmul(
                                    pS[:, 0:128], lhsT=kT_[hs, :], rhs=vtm[hs, ts, :], start=True, stop=True)), reads=[kT_, vtm], writes=[pS])
                                ec = F["eb"][:, t0 + half * 64 + 63: t0 + half * 64 + 64]
                                S.op("dve", (lambda sa=sa, sn_=sn_, ec=ec: nc.vector.scalar_tensor_tensor(
                                    out=S32[sn_][:], in0=S32[sa][:], scalar=ec, in1=pS[:, 0:128], op0=ALU.mult, op1=ALU.add)),
                                     reads=[S32[sa], F["eb"], pS], writes=[S32[sn_]])
                                S.op("act", (lambda sn_=sn_: nc.scalar.copy(out=Sbf[sn_][:], in_=S32[sn_][:])), reads=[S32[sn_]],
                                     writes=[Sbf[sn_]])
                            S.op("act", (lambda t0=t0: nc.scalar.copy(out=F["o"][:, t0:t0 + 128], in_=pso[:, 0:128])), reads=[pso], writes=[F["o"]])
                        if sidx % 2 == 1:
                            raise AssertionError
                        S.op("act", lambda: nc.scalar.activation(out=F["sq"][:], in_=F["o"][:], func=AF.Square), reads=[F["o"]], writes=[F["sq"]])
                        S.op("pe", lambda: nc.tensor.matmul(pq[:], lhsT=onesD[:], rhs=F["sq"][:], start=True, stop=True),
                             reads=[onesD, F["sq"]], writes=[pq])
                        fence(pq[:], 128, TB, [], [pq])
                        S.op("act", lambda: nc.scalar.activation(out=F["r"][:], in_=pq[:], func=AF.Sqrt, bias=epsc[:, 0:1]),
                             reads=[pq, epsc], writes=[F["r"]])
                        S.op("dve", lambda: nc.vector.reciprocal(out=F["r"][:], in_=F["r"][:]), reads=[F["r"]], writes=[F["r"]])
                        S.op("dve", lambda: nc.vector.tensor_tensor(out=F["o"][:], in0=F["o"][:], in1=F["r"][:], op=ALU.mult),
                             reads=[F["o"], F["r"]], writes=[F["o"]])
                        S.op("dve", lambda: nc.vector.tensor_tensor(out=F["o"][:], in0=F["o"][:], in1=F["gs"][:], op=ALU.mult),
                             reads=[F["o"], F["gs"]], writes=[F["o"]])
                        S.op("act", (lambda ob=ob: nc.scalar.activation(out=ob[:], in_=F["o"][:], func=AF.Identity,
                                                                         scale=vcol("hg_norm", j))), reads=[F["o"], V], writes=[ob])
                        dma("sp", actT[hd, :, bs], ob[:], [ob], [DB("act", hd)])
            proj_phase(i, w_out[j], D, glu=False)

        ident_bf = sb(es, "ident_bf", [128, 128], BF16)
        S.op("dve", lambda: nc.vector.tensor_copy(out=ident_bf[:], in_=ident), reads=[C], writes=[ident_bf])

        def moe_layer(i):
            with scope() as e1:
                GT = sb(e1, "moeGT", [NE, L])
                with scope() as e2:
                    rstd = rstd_phase(e2, xT)
                    xt = [sb(e2, f"mx{k}", [128, TB]) for k in range(3)]
                    h2f = sb(e2, "mh2f", [128, NCH, TB])
                    h2b = [sb(e2, f"mh2b{k}", [128, NCH, TB], BF16) for k in range(2)]
                    wr = sb(e2, "mwr", [128, NCH, NE])
                    rb = sb(e2, "mrb", [1, DEPTH * NE])
                    lg = sb(e2, "mlg", [128, NE])
                    t8 = sb(e2, "mt8", [128, 8])
                    mk = sb(e2, "mmk", [128, NE])
                    ex = sb(e2, "mex", [128, NE])
                    sc = sb(e2, "msc", [128, 4])
                    pl = ps(e2, "mpl", [128, NE])
                    pt = ps(e2, "mpt", [NE, 128])
                    dma("sp", wr[:], router_w[i].rearrange("(k p) e -> p k e", p=128), [], [wr])
                    dma("sp", rb[:], router_b[:, :], [], [rb])
                    n = 0
                    for blk in range(NB):
                        bs = slice(blk * TB, (blk + 1) * TB)
                        hbb = h2b[blk % 2]
                        for c in range(NCH):
                            x = xt[n % 3]
                            n += 1
                            dma("sp", x[:], xT[c, :, bs], [DB("x", c)], [x])
                            S.op("dve", (lambda x=x, bs=bs: nc.vector.tensor_tensor(out=x[:], in0=x[:], in1=rstd[:, bs], op=ALU.mult)),
                                 reads=[x, rstd], writes=[x])
                            S.op("act", (lambda x=x, c=c: nc.scalar.activation(out=h2f[:, c, :], in_=x[:], func=AF.Identity,
                                                                                scale=acol(i, 1, c), bias=modc(i, 3, c))),
                                 reads=[x, AB, MOD], writes=[h2f])
                        S.op("act", (lambda hbb=hbb: nc.scalar.copy(out=hbb[:], in_=h2f[:])), reads=[h2f], writes=[hbb])
                        dma("sp", hT[:, :, bs].rearrange("k p t -> p k t"), hbb[:], [hbb], [DB("h", c) for c in range(NCH)])
                        for ts in range(4):
                            t0 = ts * 128
                            for k in range(NCH):
                                S.op("pe", (lambda k=k, t0=t0: nc.tensor.matmul(pl[:, 0:NE], lhsT=h2f[:, k, t0:t0 + 128], rhs=wr[:, k, :],
                                                                                start=(k == 0), stop=False)), reads=[h2f, wr], writes=[pl])
                            S.op("pe", lambda: nc.tensor.matmul(pl[:, 0:NE], lhsT=ones[0:1, :], rhs=rb[0:1, i * NE:(i + 1) * NE], start=False,
                                                                stop=True), reads=[C, rb], writes=[pl])
                            fence(pl[:, 0:NE], 128, NE, [], [pl])
                            S.op("act", lambda: nc.scalar.copy(out=lg[:], in_=pl[:, 0:NE]), reads=[pl], writes=[lg])
                            S.op("dve", lambda: nc.vector.max(out=t8[:], in_=lg[:]), reads=[lg], writes=[t8])
                            S.op("dve", lambda: nc.vector.tensor_scalar(mk[:], lg[:], t8[:, 3:4], None, op0=ALU.is_ge), reads=[lg, t8], writes=[mk])
                            S.op("dve", lambda: nc.vector.tensor_scalar(sc[:, 0:1], t8[:, 0:1], -1.0, None, op0=ALU.mult), reads=[t8], writes=[sc])
                            S.op("act", lambda: nc.scalar.activation(out=ex[:], in_=lg[:], func=AF.Exp, bias=sc[:, 0:1]), reads=[lg, sc], writes=[ex])
                            S.op("dve", lambda: nc.vector.tensor_tensor(out=ex[:], in0=ex[:], in1=mk[:], op=ALU.mult), reads=[ex, mk], writes=[ex])
                            S.op("dve", lambda: nc.vector.reduce_sum(out=sc[:, 1:2], in_=ex[:], axis=AX.X), reads=[ex], writes=[sc])
                            S.op("dve", lambda: nc.vector.reciprocal(out=sc[:, 2:3], in_=sc[:, 1:2]), reads=[sc], writes=[sc])
                            S.op("dve", lambda: nc.vector.tensor_scalar(ex[:], ex[:], sc[:, 2:3], None, op0=ALU.mult), reads=[ex, sc], writes=[ex])
                            S.op("pe", lambda: nc.tensor.matmul(pt[0:NE, 0:128], lhsT=ex[:], rhs=ident, start=True, stop=True), reads=[ex, C], writes=[pt])
                            fence(pt[0:NE, 0:128], NE, 128, [], [pt])
                            tok = slice(blk * TB + t0, blk * TB + t0 + 128)
                            S.op("act", (lambda tok=tok: nc.scalar.copy(out=GT[:, tok], in_=pt[0:NE, 0:128])), reads=[pt], writes=[GT])
                Wg = [sb(e1, f"moWg{k}", [128, NCH, 2 * FF], BF16) for k in range(2)]
                Wd = [sb(e1, f"moWd{k}", [128, 4, D], BF16) for k in range(2)]
                hbk = [sb(e1, f"mohb{k}", [128, NCH, TB], BF16) for k in range(2)]
                act = [sb(e1, f"moact{k}", [128, 4, TB], BF16) for k in range(2)]
                sels = [sb(e1, f"mosel{k}", [NE, 128]) for k in range(2)]
                BU1 = sb(e1, "moBU1", [128, NE * 8])
                S.op("dve", lambda: nc.vector.tensor_scalar(BU1[:], vcol("b_gu", i * NE * 8, NE * 8), 1.0, None, op0=ALU.add),
                     reads=[V], writes=[BU1])
                gtt = [sb(e1, f"mogt{k}", [128, TB]) for k in range(2)]
                sgt = [sb(e1, f"mosg{k}", [128, TB]) for k in range(2)]
                upt = [sb(e1, f"moup{k}", [128, TB]) for k in range(2)]
                yt = [sb(e1, f"moy{k}", [128, TB]) for k in range(4)]
                pG = [ps(e1, f"mopG{k}") for k in range(2)]
                pU = [ps(e1, f"mopU{k}") for k in range(2)]
                pB = ps(e1, "mopB")
                pY = [ps(e1, f"mopY{k}") for k in range(3)]
                nb = 0
                nj = 0
                ny = 0
                with scope() as e2:
                    bd = sb(e2, "mobd", [NE, D])
                    dma("sp", bd[:], b_dn[LI[i]], [], [bd])
                    for dc in range(NCH):
                        for blk in range(NB):
                            bs = slice(blk * TB, (blk + 1) * TB)
                            py = pY[ny % 3]
                            y = yt[ny % 4]
                            ny += 1
                            S.op("pe", (lambda dc=dc, py=py, bs=bs: nc.tensor.matmul(py[:], lhsT=bd[:, dc * 128:(dc + 1) * 128], rhs=GT[:, bs],
                                                                                    start=True, stop=True)), reads=[bd, GT], writes=[py])
                            fence(py[:], 128, TB, [], [py])
                            S.op("act", (lambda y=y, py=py: nc.scalar.copy(out=y[:], in_=py[:])), reads=[py], writes=[y])
                            dma("sp", ymoe[dc, :, bs], y[:], [y], [DB("ym", dc, blk)])
                def load_expert(e):
                    wg_, wd_ = Wg[e % 2], Wd[e % 2]
                    for q in range(4):
                        dma("pool", wg_[:, 4 * q:4 * q + 4, :],
                            w_gu[LI[i], e, q * 512:(q + 1) * 512, :].rearrange("(k p) n -> p k n", p=128), [], [wg_])
                    dma("pool", wd_[:], w_dn[LI[i], e].rearrange("(k p) n -> p k n", p=128), [], [wd_])

                load_expert(0)
                NY = [ny]
                pend_dn = []
                for e in range(NE):
                    wg = Wg[e % 2]
                    wd = Wd[e % 2]
                    sel = sels[e % 2]
                    S.op("dve", (lambda e=e, sel=sel: nc.vector.tensor_scalar(sel[:], ones[0:NE, :], ident[0:NE, e:e + 1], None,
                                                                             op0=ALU.mult)), reads=[C], writes=[sel])
                    for blk in range(NB):
                        bs = slice(blk * TB, (blk + 1) * TB)
                        hb = hbk[nb % 2]
                        ac = act[nb % 2]
                        nb += 1
                        dma("sp", hb[:], hT[:, :, bs].rearrange("k p t -> p k t"), [DB("h", c) for c in range(NCH)], [hb])
                        if blk == min(1, NB - 1) and e + 1 < NE:
                            load_expert(e + 1)
                        S.op("pe", (lambda sel=sel, bs=bs: nc.tensor.matmul(pB[:], lhsT=sel[:], rhs=GT[:, bs], start=True,
                                                                      stop=True)), reads=[sel, GT], writes=[pB])
                        fence(pB[:], 128, TB, [], [pB])
                        for jq in range(4):
                            k2 = nj % 2
                            nj += 1
                            for k in range(NCH):
                                S.op("pe", (lambda wg=wg, k=k, jq=jq, hb=hb, k2=k2: nc.tensor.matmul(
                                    pG[k2][:], lhsT=wg[:, k, jq * 128:(jq + 1) * 128], rhs=hb[:, k, :], start=(k == 0), stop=(k == NCH - 1))),
                                     reads=[wg, hb], writes=[pG[k2]])
                            for k in range(NCH):
                                S.op("pe", (lambda wg=wg, k=k, jq=jq, hb=hb, k2=k2: nc.tensor.matmul(
                                    pU[k2][:], lhsT=wg[:, k, FF + jq * 128: FF + (jq + 1) * 128], rhs=hb[:, k, :], start=(k == 0),
                                    stop=(k == NCH - 1))), reads=[wg, hb], writes=[pU[k2]])
                            bgc = vcol("b_gu", (i * NE + e) * 8 + jq)
                            buc = vcol("b_gu", (i * NE + e) * 8 + 4 + jq)
                            g_, s_, u_ = gtt[k2], sgt[k2], upt[k2]
                            bu1c = BU1[:, e * 8 + 4 + jq: e * 8 + 4 + jq + 1]
                            S.op("dve", (lambda g_=g_, k2=k2, bgc=bgc: nc.vector.tensor_scalar(g_[:], pG[k2][:], bgc, 7.0, op0=ALU.add, op1=ALU.min)),
                                 reads=[pG[k2], V], writes=[g_])
                            S.op("act", (lambda g_=g_, s_=s_: nc.scalar.activation(out=s_[:], in_=g_[:], func=AF.Sigmoid, scale=1.702)),
                                 reads=[g_], writes=[s_])
                            S.op("dve", (lambda u_=u_, k2=k2, bu1c=bu1c: nc.vector.tensor_scalar(u_[:], pU[k2][:], bu1c, 8.0, op0=ALU.add, op1=ALU.min)),
                                 reads=[pU[k2], BU1], writes=[u_])
                            S.op("dve", (lambda g_=g_, s_=s_: nc.vector.tensor_tensor(out=g_[:], in0=g_[:], in1=s_[:], op=ALU.mult)),
                                 reads=[g_, s_], writes=[g_])
                            S.op("dve", (lambda g_=g_, u_=u_: nc.vector.scalar_tensor_tensor(out=g_[:], in0=u_[:], scalar=-6.0, in1=g_[:],
                                                                                             op0=ALU.max, op1=ALU.mult)),
                                 reads=[g_, u_], writes=[g_])
                            S.op("dve", (lambda g_=g_, ac=ac, jq=jq: nc.vector.tensor_tensor(out=ac[:, jq, :], in0=g_[:], in1=pB[:], op=ALU.mult)),
                                 reads=[g_, pB], writes=[ac])
                        def down_stage(wd=wd, ac=ac, bs=bs, blk=blk):
                            nonlocal_ny = NY
                            for dc in range(NCH):
                                py = pY[nonlocal_ny[0] % 3]
                                y = yt[nonlocal_ny[0] % 4]
                                nonlocal_ny[0] += 1
                                for jq in range(4):
                                    S.op("pe", (lambda wd=wd, jq=jq, dc=dc, ac=ac, py=py: nc.tensor.matmul(
                                        py[:], lhsT=wd[:, jq, dc * 128:(dc + 1) * 128], rhs=ac[:, jq, :], start=(jq == 0),
                                        stop=(jq == 3))), reads=[wd, ac], writes=[py])
                                S.op("act", (lambda y=y, py=py: nc.scalar.copy(out=y[:], in_=py[:])), reads=[py], writes=[y])
                                dma("pool", ymoe[dc, :, bs], y[:], [y], [DB("ym", dc, blk)], accum_op=ALU.add)

                        if pend_dn:
                            pend_dn.pop()()
                        pend_dn.append(down_stage)
                if pend_dn:
                    pend_dn.pop()()
                xt = [sb(e1, f"morx{k}", [128, TB]) for k in range(3)]
                yy = [sb(e1, f"mory{k}", [128, TB]) for k in range(3)]
                n = 0
                for c in range(NCH):
                    for blk in range(NB):
                        bs = slice(blk * TB, (blk + 1) * TB)
                        x = xt[n % 3]
                        y = yy[n % 3]
                        n += 1
                        dma("sp", x[:], xT[c, :, bs], [DB("x", c)], [x])
                        dma("sp", y[:], ymoe[c, :, bs], [DB("ym", c, blk)], [y])
                        S.op("dve", (lambda x=x, y=y, c=c: nc.vector.scalar_tensor_tensor(out=x[:], in0=y[:], scalar=modc(i, 5, c), in1=x[:],
                                                                                          op0=ALU.mult, op1=ALU.add)), reads=[x, y, MOD], writes=[x])
                        dma("sp", xT[c, :, bs], x[:], [x], [DB("x", c)])

        S.flush()
        for t_ in FREEZE + [epsc, ident_bf]:
            t_.b.frozen = True
            t_.b.r = []
        for i in layers:
            if i % 2 == 0:
                s5_layer(i)
            else:
                hg_layer(i)
            if "nomoe" not in dbg:
                moe_layer(i)

        with scope() as e1:
            rstd = rstd_phase(e1, xT)
            xt = [sb(e1, f"fx{k}", [128, TB]) for k in range(3)]
            n = 0
            for c in range(NCH):
                for blk in range(NB):
                    bs = slice(blk * TB, (blk + 1) * TB)
                    x = xt[n % 3]
                    n += 1
                    dma("sp", x[:], xT[c, :, bs], [DB("x", c)], [x])
                    S.op("dve", (lambda x=x, bs=bs, c=c: nc.vector.scalar_tensor_tensor(out=x[:], in0=x[:], scalar=vcol("norm_final", c),
                                                                                        in1=rstd[:, bs], op0=ALU.mult, op1=ALU.mult)),
                         reads=[x, rstd, V], writes=[x])
                    dma("sp", out[c, :, bs], x[:], [x], [DB("out", c)])
        S.finish()
    return nc


def _fm(v):
    v = np.asarray(v, np.float32)
    lead = v.shape[:-1]
    return v.reshape(*lead, NCH, 128).transpose(len(lead) + 1, *range(len(lead)), len(lead)).reshape(128, -1)


def pack_shared(inp, layers=tuple(range(DEPTH)), nomoe=False):
    f = lambda k: np.asarray(inp[k], np.float32)
    ls = list(layers)
    vec = np.zeros((128, VOFF["_n"]), np.float32)

    def put(name, arr):
        vec[:, VOFF[name]:VOFF[name] + arr.shape[1]] = arr

    put("ada_b", f("ada_b").reshape(DEPTH, 6 * NCH, 128).transpose(2, 0, 1).reshape(128, -1))
    put("norm_mix", _fm(f("norm_mix")))
    put("norm_ffn", _fm(f("norm_ffn")))
    put("norm_final", _fm(f("norm_final")))
    put("s5_d", _fm(f("s5_d")))
    put("lb_raw", _fm(f("hg_lb_raw")))
    put("hg_norm", f("hg_norm").T.copy())
    put("b_gu", f("moe_b_gate_up").reshape(DEPTH, NE, 8, 128).transpose(3, 0, 1, 2).reshape(128, -1))
    dup = lambda a: np.concatenate([a, a], axis=2).transpose(2, 0, 1).reshape(128, -1)
    put("s5_lre", dup(f("s5_lambda_re")))
    put("s5_lim", dup(f("s5_lambda_im")))
    put("s5_ldt", np.broadcast_to(f("s5_log_dt").reshape(1, -1), (128, 256)).copy())
    bre, bim = f("s5_b_re"), f("s5_b_im")
    cre, cim = f("s5_c_re"), f("s5_c_im")

    def stackB(a, b):
        return np.concatenate([a, b], axis=2).transpose(0, 2, 1, 3).reshape(2, 128, -1)

    def stackC(a, b):
        return np.concatenate([a, b], axis=3).transpose(0, 3, 1, 2).reshape(2, 128, -1)

    s5B = np.stack([stackB(bre, bim), stackB(bim, bre)], axis=1)
    s5C = np.stack([stackC(cre, cim), stackC(cim, cre)], axis=1)
    shared = {
        "cst": CONST_NP,
        "ada_w": f("ada_w") if len(ls) == DEPTH else np.ascontiguousarray(f("ada_w")[ls]),
        "s5B": np.ascontiguousarray(s5B), "s5C": np.ascontiguousarray(s5C),
        "s5_w_glu": f("s5_w_glu"), "hg_w_in": f("hg_w_in"), "hg_w_out": f("hg_w_out"),
        "router_w": f("router_w"), "router_b": f("router_b").reshape(1, -1),
    }
    if nomoe:
        shared.update({"moe_w_gate_up": np.zeros((1, 1, 1, 1), np.float32), "moe_w_down": np.zeros((1, 1, 1, 1), np.float32),
                       "moe_b_down": np.zeros((1, 1, 1), np.float32)})
    elif len(ls) == DEPTH:
        shared.update({"moe_w_gate_up": f("moe_w_gate_up"), "moe_w_down": f("moe_w_down"), "moe_b_down": f("moe_b_down")})
    else:
        shared.update({"moe_w_gate_up": np.ascontiguousarray(f("moe_w_gate_up")[ls]),
                       "moe_w_down": np.ascontiguousarray(f("moe_w_down")[ls]),
                       "moe_b_down": np.ascontiguousarray(f("moe_b_down")[ls])})
    return vec, shared


def core_inputs(inp, b, vec, shared, L):
    v = vec.copy()
    v[:, VOFF["cT"]:VOFF["cT"] + NCH] = np.asarray(inp["c"], np.float32)[b].reshape(NCH, 128).T
    x = np.asarray(inp["x"], np.float32)[b, :L]
    m = dict(shared)
    m["vec"] = v
    m["xT"] = np.ascontiguousarray(x.T.reshape(NCH, 128, L))
    return m


_NC_CACHE = {}


def kernel(**inputs):
    B, L, _ = inputs["x"].shape
    vec, shared = pack_shared(inputs)
    if L not in _NC_CACHE:
        _NC_CACHE[L] = build(L)
    nc = _NC_CACHE[L]
    in_maps = [core_inputs(inputs, b, vec, shared, L) for b in range(B)]
    res = run_bass_kernel_spmd(nc, in_maps, core_ids=list(range(B)))
    outs = [np.asarray(r["outT"]).reshape(D, L).T for r in res.results]
    return np.ascontiguousarray(np.stack(outs, axis=0).astype(np.float32))
```
